# Optimizing a Trainium2 kernel written in Bass

```python
import math
import jax, jax.numpy as jnp
from jax import lax
import numpy as np

D_MODEL = 1024
BATCH = 4
SEQ = 4096
DEPTH = 1

MEM_LEN = 256
D_MIX = D_MODEL
D_DIFF = D_MIX // 2
D_HGRN = D_MIX // 4
D_XMEM = D_MIX // 4
H_DIFF = 4
DH_DIFF = D_DIFF // (2 * H_DIFF)
H_HGRN = 4
DK_HGRN = D_HGRN // H_HGRN
H_XMEM = 4
DH_XMEM = D_XMEM // H_XMEM
D_IN_PROJ = 3 * D_DIFF + 5 * D_HGRN + D_XMEM
Q_BLOCK = 128
HGRN_CHUNK = 16
N_EXPERTS = 32
TOP_K = 4
D_EXPERT = D_MODEL
SWIGLU_ALPHA = 1.702
SWIGLU_LIMIT = 7.0
MOE_BLOCK = 256
NORM_EPS = 1e-5

kernel_name = "hybrid_diffattn_hgrn2_memxattn_moe_encoder"


def layer_norm(t, g, b):
    t32 = t.astype(jnp.float32)
    mu = jnp.mean(t32, axis=-1, keepdims=True)
    var = jnp.mean(jnp.square(t32 - mu), axis=-1, keepdims=True)
    return ((t32 - mu) * lax.rsqrt(var + NORM_EPS) * g + b).astype(t.dtype)


def rms_norm(t):
    t32 = t.astype(jnp.float32)
    return t32 * lax.rsqrt(jnp.mean(jnp.square(t32), axis=-1, keepdims=True) + NORM_EPS)


def alibi_slopes(n_heads):
    return jnp.asarray(2.0 ** (-8.0 * np.arange(1, n_heads + 1) / n_heads), dtype=jnp.float32)


def diff_attention(q, k, v, lam):
    B, H, _, S, Dh = q.shape
    nb = S // Q_BLOCK
    scale = 1.0 / math.sqrt(Dh)
    slopes = alibi_slopes(H)
    kpos = jnp.arange(S, dtype=jnp.int32)
    qb = jnp.moveaxis(q.reshape(B, H, 2, nb, Q_BLOCK, Dh), 3, 0)

    def block(args):
        q_blk, i = args
        qpos = i * Q_BLOCK + jnp.arange(Q_BLOCK, dtype=jnp.int32)
        dist = jnp.abs(qpos[:, None] - kpos[None, :]).astype(jnp.float32)
        bias = -slopes[:, None, None] * dist
        s = jnp.einsum('bhmqd,bhmkd->bhmqk', q_blk, k).astype(jnp.float32) * scale
        p = jax.nn.softmax(s + bias[None, :, None], axis=-1)
        p = p[:, :, 0] - lam * p[:, :, 1]
        return jnp.einsum('bhqk,bhkd->bhqd', p.astype(v.dtype), v)

    out = lax.map(block, (qb, jnp.arange(nb, dtype=jnp.int32)))
    return jnp.moveaxis(out, 0, 2).reshape(B, H, S, 2 * Dh)


def hgrn2_chunkwise(q, k, logf, v):
    B, H, S, Dk = q.shape
    Dv = v.shape[-1]
    C = HGRN_CHUNK
    N = S // C
    q = q.reshape(B, H, N, C, Dk)
    k = k.reshape(B, H, N, C, Dk).astype(jnp.float32)
    v = v.reshape(B, H, N, C, Dv)
    b = jnp.cumsum(logf.reshape(B, H, N, C, Dk).astype(jnp.float32), axis=3)
    mask = jnp.tril(jnp.ones((C, C), dtype=bool))
    rel = b[:, :, :, :, None, :] - b[:, :, :, None, :, :]
    decay = jnp.exp(jnp.where(mask[:, :, None], rel, -jnp.inf))
    A = jnp.einsum('bhntd,bhnsd,bhntsd->bhnts', q.astype(jnp.float32), k, decay)
    o_intra = jnp.einsum('bhnts,bhnsv->bhntv', A, v.astype(jnp.float32))
    b_last = b[:, :, :, -1, :]
    k_to_end = k * jnp.exp(b_last[:, :, :, None, :] - b)
    U = jnp.einsum('bhncd,bhncv->bhndv', k_to_end, v.astype(jnp.float32))
    a = jnp.exp(b_last)

    def step(S_prev, inp):
        a_n, U_n = inp
        return a_n[..., None] * S_prev + U_n, S_prev

    S0 = jnp.zeros((B, H, Dk, Dv), jnp.float32)
    _, S_before = lax.scan(step, S0, (jnp.moveaxis(a, 2, 0), jnp.moveaxis(U, 2, 0)))
    S_before = jnp.moveaxis(S_before, 0, 2)
    o_inter = jnp.einsum('bhncd,bhndv->bhncv', q.astype(jnp.float32) * jnp.exp(b), S_before)
    return (o_intra + o_inter).reshape(B, H, S, Dv)


def hybrid_mixer(h, mem, w_in, lq1, lk1, lq2, lk2, diff_norm_w, lb_f, lb_b,
                 hgrn_norm_w, w_mem_kv, w_o, layer_idx):
    B, S, _ = h.shape
    sizes = [D_DIFF] * 3 + [D_HGRN] * 5 + [D_XMEM]
    points = [int(p) for p in np.cumsum(sizes)[:-1]]
    u = h @ w_in
    dq, dk, dv, hq, hf_f, hf_b, hi, hg, mq = jnp.split(u, points, axis=-1)

    q = dq.reshape(B, S, H_DIFF, 2, DH_DIFF).transpose(0, 2, 3, 1, 4)
    k = dk.reshape(B, S, H_DIFF, 2, DH_DIFF).transpose(0, 2, 3, 1, 4)
    v = dv.reshape(B, S, H_DIFF, 2 * DH_DIFF).transpose(0, 2, 1, 3)
    lam_init = 0.8 - 0.6 * math.exp(-0.3 * layer_idx)
    lam = (jnp.exp(jnp.sum(lq1.astype(jnp.float32) * lk1.astype(jnp.float32)))
           - jnp.exp(jnp.sum(lq2.astype(jnp.float32) * lk2.astype(jnp.float32))) + lam_init)
    o = diff_attention(q, k, v, lam)
    o = rms_norm(o) * diff_norm_w * (1.0 - lam_init)
    o_diff = o.transpose(0, 2, 1, 3).reshape(B, S, D_DIFF)

    heads = lambda t: t.reshape(B, S, H_HGRN, DK_HGRN).transpose(0, 2, 1, 3)
    q_h = heads(jax.nn.silu(hq))
    v_h = heads(hi)

    def gate(f_logit, lb):
        f = lb + (1.0 - lb) * jax.nn.sigmoid(f_logit.astype(jnp.float32))
        return heads(1.0 - f), heads(jnp.log(f))

    k_f, logf_f = gate(hf_f, lb_f)
    k_b, logf_b = gate(hf_b, lb_b)
    o_fwd = hgrn2_chunkwise(q_h, k_f, logf_f, v_h)
    flip = lambda t: jnp.flip(t, axis=2)
    o_bwd = flip(hgrn2_chunkwise(flip(q_h), flip(k_b), flip(logf_b), flip(v_h)))
    o = (o_fwd + o_bwd).transpose(0, 2, 1, 3)
    o = rms_norm(o) * hgrn_norm_w.reshape(H_HGRN, DK_HGRN)
    o_hgrn = o.reshape(B, S, D_HGRN) * jax.nn.sigmoid(hg.astype(jnp.float32))

    kv = mem @ w_mem_kv
    mk, mv = jnp.split(kv, 2, axis=-1)
    M = mem.shape[1]
    mk = mk.reshape(B, M, H_XMEM, DH_XMEM).transpose(0, 2, 1, 3)
    mv = mv.reshape(B, M, H_XMEM, DH_XMEM).transpose(0, 2, 1, 3)
    mqh = mq.reshape(B, S, H_XMEM, DH_XMEM).transpose(0, 2, 1, 3)
    s = jnp.einsum('bhsd,bhmd->bhsm', mqh, mk).astype(jnp.float32) / math.sqrt(DH_XMEM)
    p = jax.nn.softmax(s, axis=-1)
    o_mem = jnp.einsum('bhsm,bhmd->bshd', p.astype(mv.dtype), mv).reshape(B, S, D_XMEM)

    cat = jnp.concatenate([o_diff.astype(h.dtype), o_hgrn.astype(h.dtype), o_mem.astype(h.dtype)], axis=-1)
    return cat @ w_o


def moe_ffn(x2d, router_w, router_b, w_gu, b_gu, w_dn, b_dn):
    T, D = x2d.shape
    TK = T * TOP_K
    logits = (x2d @ router_w + router_b).astype(jnp.float32)
    top_val, top_idx = lax.top_k(logits, TOP_K)
    gates = jax.nn.softmax(top_val, axis=-1)
    flat_e = top_idx.reshape(-1).astype(jnp.int32)
    flat_tok = jnp.repeat(jnp.arange(T, dtype=jnp.int32), TOP_K)
    order = jnp.argsort(flat_e)
    sorted_e = flat_e[order]
    sorted_tok = flat_tok[order]
    sorted_gate = gates.reshape(-1)[order]
    counts = jnp.bincount(flat_e, length=N_EXPERTS).astype(jnp.int32)
    padded = (counts + MOE_BLOCK - 1) // MOE_BLOCK * MOE_BLOCK
    start = jnp.cumsum(counts) - counts
    pend = jnp.cumsum(padded)
    pstart = pend - padded
    dest = pstart[sorted_e] + jnp.arange(TK, dtype=jnp.int32) - start[sorted_e]
    n_blocks = -(-TK // MOE_BLOCK) + N_EXPERTS
    n_rows = n_blocks * MOE_BLOCK
    row_tok = jnp.zeros((n_rows,), jnp.int32).at[dest].set(sorted_tok)
    row_gate = jnp.zeros((n_rows,), jnp.float32).at[dest].set(sorted_gate)
    block_e = jnp.minimum(
        jnp.searchsorted(pend, jnp.arange(n_blocks, dtype=jnp.int32) * MOE_BLOCK, side='right'),
        N_EXPERTS - 1)
    xs = x2d[row_tok].reshape(n_blocks, MOE_BLOCK, D)

    def expert_block(args):
        xb, e = args
        hh = xb @ w_gu[e] + b_gu[e]
        glu = jnp.minimum(hh[:, 0::2], SWIGLU_LIMIT)
        lin = jnp.clip(hh[:, 1::2], -SWIGLU_LIMIT, SWIGLU_LIMIT)
        act = glu * jax.nn.sigmoid(SWIGLU_ALPHA * glu) * (lin + 1.0)
        return act @ w_dn[e] + b_dn[e]

    ys = lax.map(expert_block, (xs, block_e)).reshape(n_rows, D).astype(jnp.float32)
    out = jnp.zeros((T, D), jnp.float32).at[row_tok].add(ys * row_gate[:, None])
    return out.astype(x2d.dtype)


def setup_inputs(seed: int = 0) -> dict:
    key = jax.random.key(seed)
    ks = jax.random.split(key, 24)
    nrm = lambda k, shape, sc: jax.random.normal(k, shape, jnp.float32) * sc
    beta = (8.0 * DEPTH) ** -0.25
    L = DEPTH
    return {
        "x": nrm(ks[0], (BATCH, SEQ, D_MODEL), 1.0),
        "mem": nrm(ks[1], (BATCH, MEM_LEN, D_MODEL), 1.0),
        "w_in": nrm(ks[2], (L, D_MODEL, D_IN_PROJ), D_MODEL ** -0.5),
        "lam_q1": nrm(ks[3], (L, DH_DIFF), 0.1),
        "lam_k1": nrm(ks[4], (L, DH_DIFF), 0.1),
        "lam_q2": nrm(ks[5], (L, DH_DIFF), 0.1),
        "lam_k2": nrm(ks[6], (L, DH_DIFF), 0.1),
        "diff_norm_w": 1.0 + nrm(ks[7], (L, 2 * DH_DIFF), 0.01),
        "hgrn_lb_fwd": nrm(ks[8], (L + 1, D_HGRN), 1.0),
        "hgrn_lb_bwd": nrm(ks[9], (L + 1, D_HGRN), 1.0),
        "hgrn_norm_w": 1.0 + nrm(ks[10], (L, D_HGRN), 0.01),
        "w_mem_kv": nrm(ks[11], (L, D_MODEL, 2 * D_XMEM), D_MODEL ** -0.5),
        "w_o": nrm(ks[12], (L, D_MIX, D_MODEL), D_MIX ** -0.5 * beta),
        "ln1_g": 1.0 + nrm(ks[13], (L, D_MODEL), 0.01),
        "ln1_b": nrm(ks[14], (L, D_MODEL), 0.01),
        "router_w": nrm(ks[15], (L, D_MODEL, N_EXPERTS), D_MODEL ** -0.5),
        "router_b": nrm(ks[16], (L, N_EXPERTS), 0.01),
        "w_gate_up": nrm(ks[17], (L, N_EXPERTS, D_MODEL, 2 * D_EXPERT), D_MODEL ** -0.5),
        "b_gate_up": nrm(ks[18], (L, N_EXPERTS, 2 * D_EXPERT), 0.01),
        "w_down": nrm(ks[19], (L, N_EXPERTS, D_EXPERT, D_MODEL), D_EXPERT ** -0.5 * beta),
        "b_down": nrm(ks[20], (L, N_EXPERTS, D_MODEL), 0.01),
        "ln2_g": 1.0 + nrm(ks[21], (L, D_MODEL), 0.01),
        "ln2_b": nrm(ks[22], (L, D_MODEL), 0.01),
    }


def reference(x, mem, w_in, lam_q1, lam_k1, lam_q2, lam_k2, diff_norm_w, hgrn_lb_fwd,
              hgrn_lb_bwd, hgrn_norm_w, w_mem_kv, w_o, ln1_g, ln1_b, router_w, router_b,
              w_gate_up, b_gate_up, w_down, b_down, ln2_g, ln2_b):
    B, S, D = x.shape
    alpha = (2.0 * DEPTH) ** 0.25
    lb_fwd_all = jnp.cumsum(jax.nn.softmax(hgrn_lb_fwd.astype(jnp.float32), axis=0), axis=0)
    lb_bwd_all = jnp.cumsum(jax.nn.softmax(hgrn_lb_bwd.astype(jnp.float32), axis=0), axis=0)
    h = x
    for l in range(DEPTH):
        mix = hybrid_mixer(h, mem, w_in[l], lam_q1[l], lam_k1[l], lam_q2[l], lam_k2[l],
                           diff_norm_w[l], lb_fwd_all[l], lb_bwd_all[l], hgrn_norm_w[l],
                           w_mem_kv[l], w_o[l], l)
        h = layer_norm(alpha * h + mix, ln1_g[l], ln1_b[l])
        ffn = moe_ffn(h.reshape(B * S, D), router_w[l], router_b[l], w_gate_up[l],
                      b_gate_up[l], w_down[l], b_down[l]).reshape(B, S, D)
        h = layer_norm(alpha * h + ffn, ln2_g[l], ln2_b[l])
    return h
```

```python
import os
import numpy as np
import ml_dtypes
import concourse.bass as bass
import concourse.mybir as mybir
from concourse.bass_utils import run_bass_kernel_spmd

F32 = mybir.dt.float32
BF16 = mybir.dt.bfloat16
I32 = mybir.dt.int32
ALU = mybir.AluOpType
AF = mybir.ActivationFunctionType
AX = mybir.AxisListType

ENGS = ("pe", "act", "dve", "pool", "sp")
EPOCH = 8192
NDMASEM = 8
NCORES = 8
CAP = 384
NEXP = 32
ALPHA = 2.0 ** 0.25
EPS = 1e-5


class Sched:
    def __init__(self, nc, same_engine_sync=True):
        self.nc = nc
        self.q = {e: [] for e in ENGS}
        self.count = {e: 0 for e in ENGS}
        self.dcount = {e: 0 for e in ENGS}
        self.last_write = {}
        self.readers = {}
        self.waited_eng = {e: {} for e in ENGS}
        self.waited_dma = {e: {} for e in ENGS}
        self.same = same_engine_sync
        self.sems = {}
        self.final_events = []
        self.pending = {e: [] for e in ENGS}

    def _deps_for(self, eng, reads, writes):
        evs = list(self.pending[eng])
        self.pending[eng] = []
        for k in reads:
            w = self.last_write.get(k)
            if w is not None:
                evs.append(w)
            if isinstance(k, str) and k[0] == "B" and k[1:].isdigit():
                evs.extend(r for r in self.readers.get(k, ()) if r[1] != eng)
        for k in writes:
            w = self.last_write.get(k)
            if w is not None:
                evs.append(w)
            evs.extend(self.readers.get(k, ()))
        return evs

    def _reduce(self, eng, evs):
        waits = []
        best = {}
        for ev in evs:
            if ev[0] == "eng":
                _, e, idx = ev
                if e == eng and (not self.same or e == "pe"):
                    continue
                if self.waited_eng[eng].get(e, -1) >= idx:
                    continue
                if best.get(e, -1) < idx:
                    best[e] = idx
            else:
                _, e, d = ev
                if self.waited_dma[eng].get((e, d % NDMASEM), -1) >= d:
                    continue
                self.waited_dma[eng][(e, d % NDMASEM)] = d
                waits.append((("dma", e, d % NDMASEM), 16 * (d // NDMASEM + 1)))
        for e, idx in best.items():
            self.waited_eng[eng][e] = idx
            waits.append((("eng", e, idx // EPOCH), idx % EPOCH + 1))
        return waits

    def _record(self, ev, reads, writes):
        for k in reads:
            self.readers.setdefault(k, []).append(ev)
        for k in writes:
            self.last_write[k] = ev
            self.readers[k] = []

    def op(self, eng, fn, reads=(), writes=()):
        evs = self._deps_for(eng, reads, writes)
        waits = self._reduce(eng, evs)
        idx = self.count[eng]
        self.count[eng] += 1
        self.q[eng].append((fn, waits, (("eng", eng, idx // EPOCH), 1)))
        ev = ("eng", eng, idx)
        self._record(ev, reads, writes)
        return ev

    def dma(self, eng, fn, reads=(), writes=(), final=False):
        evs = self._deps_for(eng, reads, writes)
        d = self.dcount[eng]
        self.dcount[eng] += 1
        if d >= NDMASEM:
            evs.append(("dma", eng, d - NDMASEM))
        waits = self._reduce(eng, evs)
        self.q[eng].append((fn, waits, (("dma", eng, d % NDMASEM), 16)))
        ev = ("dma", eng, d)
        self._record(ev, reads, writes)
        if final:
            self.final_events.append(ev)
        return ev

    def barrier(self):
        evs = []
        for e in ENGS:
            if self.count[e] > 0:
                evs.append(("eng", e, self.count[e] - 1))
            for d in range(max(0, self.dcount[e] - NDMASEM), self.dcount[e]):
                evs.append(("dma", e, d))
        for e in ENGS:
            self.pending[e].extend(evs)
        self.last_write.clear()
        self.readers.clear()

    def emit(self):
        nc = self.nc
        from contextlib import ExitStack
        keys = set()
        for e in ENGS:
            for fn, waits, inc in self.q[e]:
                keys.add(inc[0])
                for k, v in waits:
                    keys.add(k)
        fin = self._reduce("sp", self.final_events)
        for k, v in fin:
            keys.add(k)
        with ExitStack() as st:
            for k in sorted(keys, key=str):
                self.sems[k] = st.enter_context(nc.semaphore("s_" + "_".join(str(x) for x in k)))
            st.enter_context(nc.allow_low_precision(reason="bf16 matmul operands, fp32 accumulation"))
            block = st.enter_context(nc.Block())
            engmap = {"pe": block.tensor, "act": block.scalar, "dve": block.vector,
                      "pool": block.gpsimd, "sp": block.sync}
            for e in ENGS:
                items = self.q[e]
                extra = fin if e == "sp" else []

                def body(engine, items=items, extra=extra):
                    for fn, waits, inc in items:
                        for k, v in waits:
                            engine.wait_ge(self.sems[k], v)
                        ins = fn(engine)
                        ins.then_inc(self.sems[inc[0]], inc[1])
                    for k, v in extra:
                        engine.wait_ge(self.sems[k], v)
                if items or extra:
                    engmap[e](body)


class V:
    __slots__ = ("ap", "keys")

    def __init__(self, ap, keys):
        self.ap = ap
        self.keys = tuple(keys)

    def __getitem__(self, idx):
        return V(self.ap[idx], self.keys)

    def k(self, *suffix):
        return V(self.ap, [(self.keys[0],) + tuple(suffix)])

    def re(self, s, **kw):
        return V(self.ap.rearrange(s, **kw), self.keys)

    def bc(self, axis, shape):
        return V(self.ap.unsqueeze(axis).broadcast_to(list(shape)), self.keys)

    def bitcast(self, dt):
        return V(self.ap.bitcast(dt), self.keys)


_DTSIZE = {F32: 4, BF16: 2, I32: 4}


class Arena:
    def __init__(self, nc, base=16512, limit=229376 - 256):
        self.nc = nc
        self.off = base
        self.limit = limit
        self.n = 0

    def alloc(self, name, shape, dtype):
        size = int(np.prod(shape[1:])) * _DTSIZE[dtype]
        self.off = (self.off + 63) // 64 * 64
        assert self.off + size <= self.limit, f"SBUF overflow at {name}: {self.off + size}"
        self.n += 1
        t = self.nc.alloc_sbuf_tensor_at(f"{name}_{self.n}", list(shape), dtype, offset=self.off)
        self.off += size
        return V(t.ap(), [name])

    def mark(self):
        return self.off

    def release(self, m):
        self.off = m


def build_program(stage=99, dbg=False):
    nc = bass.Bass("TRN2", target_bir_lowering=False)
    S = Sched(nc)

    def din(name, shape, dt=F32):
        return V(nc.dram_tensor(name, list(shape), dt, kind="ExternalInput").ap(), [])

    xT = din("xT", [1024, 4096])
    x_own = din("x_own", [2048, 1024])
    memT = din("memT", [1024, 256])
    w_h = din("w_h", [1024, 1280])
    w_a = din("w_a", [1024, 1792])
    lbp = din("lbp", [4, 256])
    hnw = din("hnw", [64, 4])
    lamv = din("lamv", [4, 64])
    dnw = din("dnw", [128, 1])
    w_mem = din("w_mem", [1024, 512])
    w_o = din("w_o", [1024, 1024])
    lnp = din("lnp", [4, 1024])
    router_w = din("router_w", [1024, 32])
    router_b = din("router_b", [1, 32])
    NE_D = NEXP if stage >= 3 else 1
    w_gu = din("w_gu", [NE_D, 1024, 2048])
    b_gu = din("b_gu", [128, NEXP * 16])
    w_dn = din("w_dn", [NE_D, 1024, 1024])
    b_dn = din("b_dn", [NEXP, 1024])
    cst = din("cst", [128, 1024])
    dstrip = din("dstrip", [128, 6016])
    qaug_d = din("qaug", [128, 2048])
    btab_d = din("btab", [128, 256])
    sgn_d = din("sgn", [128, 256])
    out = V(nc.dram_tensor("out", [2048, 1024], F32, kind="ExternalOutput").ap(), ["out"])
    dbg_o = None
    if dbg:
        dbg_o = V(nc.dram_tensor("dbg", [128, 16384], F32, kind="ExternalOutput").ap(), ["dbg"])
    Xs = V(nc.dram_tensor("Xs", [NEXP * CAP, 1024], BF16, kind="Internal").ap(), ["Xs"])
    Ys = V(nc.dram_tensor("Ys", [NEXP * CAP, 1024], BF16, kind="Internal").ap(), ["Ys"])
    Hf = V(nc.dram_tensor("Hf", [2048, 1024], F32, kind="Internal").ap(), ["Hf"])

    A = Arena(nc)
    banks = []
    for i in range(8):
        t = nc.alloc_psum_tensor(f"bank{i}", [128, 512], F32)
        banks.append(V(t.ap(), [f"B{i}"]))

    _breg = {}

    def bcheck(e):
        if "r" not in _breg:
            _breg["r"] = e.to_reg(NEXP * CAP - 1)
        return _breg["r"]

    def MM(o, lhsT, rhs, start=True, stop=True, skip=False):
        rd = list(lhsT.keys) + list(rhs.keys) + ([] if start else list(o.keys))
        kw = {"skip_group_check": True} if skip else {}
        S.op("pe", lambda e: e.matmul(o.ap, lhsT=lhsT.ap, rhs=rhs.ap, start=start, stop=stop, **kw), rd, o.keys)

    def TR(o, in_, ident):
        S.op("pe", lambda e: e.transpose(out=o.ap, in_=in_.ap, identity=ident.ap),
             list(in_.keys) + list(ident.keys), o.keys)

    def ACT(o, in_, func, bias=None, scale=1.0):
        rd = list(in_.keys)
        kw = {}
        if isinstance(bias, V):
            rd += list(bias.keys)
            kw["bias"] = bias.ap
        elif bias is not None:
            kw["bias"] = bias
        S.op("act", lambda e: e.activation(out=o.ap, in_=in_.ap, func=func, scale=scale, **kw), rd, o.keys)

    def TT(eng, o, a, b, op):
        S.op(eng, lambda e: e.tensor_tensor(out=o.ap, in0=a.ap, in1=b.ap, op=op),
             list(a.keys) + list(b.keys), o.keys)

    def TS(eng, o, a, s1, op0, s2=None, op1=None):
        rd = list(a.keys)
        v1 = s1
        v2 = s2
        if isinstance(s1, V):
            rd += list(s1.keys)
            v1 = s1.ap
        if isinstance(s2, V):
            rd += list(s2.keys)
            v2 = s2.ap
        if op1 is None:
            S.op(eng, lambda e: e.tensor_scalar(out=o.ap, in0=a.ap, scalar1=v1, scalar2=None, op0=op0), rd, o.keys)
        else:
            S.op(eng, lambda e: e.tensor_scalar(out=o.ap, in0=a.ap, scalar1=v1, scalar2=v2, op0=op0, op1=op1),
                 rd, o.keys)

    def STT(eng, o, in0, scalar, in1, op0, op1):
        rd = list(in0.keys) + list(in1.keys)
        sv = scalar
        if isinstance(scalar, V):
            rd += list(scalar.keys)
            sv = scalar.ap
        S.op(eng, lambda e: e.scalar_tensor_tensor(out=o.ap, in0=in0.ap, scalar=sv, in1=in1.ap, op0=op0, op1=op1),
             rd, o.keys)

    def CP(eng, o, in_):
        if eng == "act":
            ACT(o, in_, AF.Copy)
        else:
            S.op(eng, lambda e: e.tensor_copy(out=o.ap, in_=in_.ap), in_.keys, o.keys)

    def MS(eng, o, val):
        S.op(eng, lambda e: e.memset(o.ap, val), (), o.keys)

    def RECIP(o, in_):
        S.op("dve", lambda e: e.reciprocal(out=o.ap, in_=in_.ap), in_.keys, o.keys)

    def DMA(q, o, in_, final=False):
        S.dma(q, lambda e: e.dma_start(out=o.ap, in_=in_.ap), in_.keys, o.keys, final=final)

    cst_f = A.alloc("cst_f", [128, 1024], F32)
    cst_b = A.alloc("cst_b", [128, 1024], BF16)
    small = A.alloc("small", [128, 64], F32)
    tokslot_t = A.alloc("tokslot", [128, 64], I32)
    tokslot = [[tokslot_t[:, ti * 4 + k:ti * 4 + k + 1].k(ti, k) for k in range(4)] for ti in range(16)]
    tokgate = A.alloc("tokgate", [128, 16, 4], F32)
    tokG = A.alloc("tokG", [128, 16, 32], F32)
    pM = A.mark()
    ohg = A.alloc("ohg", [128, 4, 2048], BF16)

    eps_t = small[:, 0:1].k("eps")
    MS("dve", eps_t, EPS)
    DMA("sp", cst_f, cst)
    DMA("pool", cst_b, cst)
    MA = cst_f[:, 0:128]
    MB = cst_f[:, 128:256]
    MAx = cst_f[:, 256:384]
    MBx = cst_f[:, 384:512]
    ident_f = cst_f[:, 512:640]
    Cm_f = cst_f[:, 768:776]
    ident_b = cst_b[:, 512:640]
    tri_b = cst_b[:, 640:768]
    ones_b = cst_b[:, 776:904]
    cmx_b_src = cst_b[:, 768:776]

    pmark = A.mark()

    wH = A.alloc("wH", [128, 8, 1280], BF16)
    xTb = [A.alloc(f"xTb{i}", [128, 8, 256], BF16) for i in range(2)]
    st_sq = A.alloc("st_sq", [128, 16, 256], BF16)
    st_hi = A.alloc("st_hi", [128, 16, 256], BF16)
    st_hfB = A.alloc("st_hfB", [128, 16, 256], F32)
    gateT = A.alloc("gateT", [64, 4, 2048], BF16)
    oacc = A.alloc("oacc", [64, 4, 2048], F32)
    lbt = A.alloc("lbt", [128, 4, 256], F32)
    hnw_t = A.alloc("hnw_t", [64, 4], F32)
    cmx = A.alloc("cmx", [128, 8, 64], BF16)
    Sall = [A.alloc(f"Sall{i}", [64, 8, 4, 64], F32) for i in range(2)]

    def wset(i):
        d = {}
        for nm in ("f", "logf", "kk", "ex", "sqf", "hq"):
            d[nm] = A.alloc(f"{nm}{i}", [128, 256], F32)
        for nm in ("Ke", "Kbf", "Qbf", "Vbf"):
            d[nm] = A.alloc(f"{nm}{i}", [128, 256], BF16)
        d["Vexp"] = A.alloc(f"Vexp{i}", [128, 4, 8, 64], BF16)
        d["QT"] = A.alloc(f"QT{i}", [128, 4, 128], BF16)
        d["KT"] = A.alloc(f"KT{i}", [128, 4, 128], BF16)
        d["ATm"] = A.alloc(f"ATm{i}", [128, 4, 128], BF16)
        d["a"] = A.alloc(f"a{i}", [64, 4, 8], F32)
        d["Usb"] = A.alloc(f"Usb{i}", [64, 4, 8, 64], F32)
        d["Sb"] = A.alloc(f"Sb{i}", [128, 8, 4, 64], BF16)
        d["tmp"] = A.alloc(f"stmp{i}", [64, 4, 64], F32)
        return d
    W = [wset(0)]
    W1 = dict(W[0])
    W1["QT"] = A.alloc("QT1", [128, 4, 128], BF16)
    W1["ATm"] = A.alloc("ATm1", [128, 4, 128], BF16)
    W1["a"] = A.alloc("a1", [64, 4, 8], F32)
    W1["Usb"] = A.alloc("Usb1", [64, 4, 8, 64], F32)
    W.append(W1)
    lbm = A.mark()
    lbraw = A.alloc("lbraw", [128, 4, 256], F32)
    A.release(lbm)
    _f = A.alloc("fin_sq", [64, 512], BF16)
    fin_sq = V(_f.ap, ["fin_sq", "lbraw"])
    _f = A.alloc("fin_ln", [64, 512], F32)
    fin_ln = V(_f.ap, ["fin_ln", "lbraw"])
    _f = A.alloc("fin_t", [64, 512], F32)
    fin_t = V(_f.ap, ["fin_t", "lbraw"])
    _f = A.alloc("geg", [64, 256], F32)
    geg = V(_f.ap, ["geg", "lbraw"])

    B0, B1, B2, B3, B4, B5, B6, B7 = banks
    B3b = B3.bitcast(BF16)

    for c in range(8):
        DMA("pool", wH[:, c, :], w_h[c * 128:(c + 1) * 128, :])
    DMA("sp", lbraw, V(lbp.ap.unsqueeze(0).broadcast_to([128, 4, 256]), []))
    DMA("sp", hnw_t, hnw)
    CP("pool", cmx, cmx_b_src.bc(2, [128, 8, 64]))
    for di in range(2):
        TT("dve", lbt[:, 2 * di, :], lbraw[:, 2 * di + 1, :], lbraw[:, 2 * di, :], ALU.subtract)
        ACT(lbt[:, 2 * di, :], lbt[:, 2 * di, :], AF.Exp)
        TS("dve", lbt[:, 2 * di, :], lbt[:, 2 * di, :], 1.0, ALU.add)
        RECIP(lbt[:, 2 * di, :], lbt[:, 2 * di, :])
        TS("dve", lbt[:, 2 * di + 1, :], lbt[:, 2 * di, :], -1.0, ALU.mult, 1.0, ALU.add)
    MS("dve", Sall[0], 0.0)
    MS("dve", Sall[1], 0.0)
    for _t in (W[0]["QT"], W[1]["QT"], W[0]["KT"], W[0]["Sb"]):
        MS("dve", _t[64:128], 0.0)
    MS("dve", ohg[64:128], 0.0)

    SCAN_ENG = os.environ.get("SCAN_ENG", "pool")

    def hgrn_early(blk, d, own, hf_src, hq_src, hi_src, sidx):
        w = W[sidx % 2]
        di = 0 if d == "A" else 1
        lb, omlb = lbt[:, 2 * di, :], lbt[:, 2 * di + 1, :]
        M_incl, M_excl = (MA, MBx) if d == "A" else (MB, MAx)
        ob = blk - 16
        ACT(w["f"], hf_src, AF.Exp)
        ACT(w["f"], w["f"], AF.Ln, bias=1.0)
        ACT(w["f"], w["f"], AF.Exp, scale=-1.0)
        TT("dve", w["kk"], w["f"], omlb, ALU.mult)
        ACT(w["logf"], w["kk"], AF.Ln, bias=1.0, scale=-1.0)
        MM(B2[:, 0:256], M_incl, w["logf"])
        MM(B2[:, 256:512], M_excl, w["logf"])
        ACT(w["ex"], B2[:, 256:512], AF.Exp)
        TT("dve", w["Ke"], w["kk"], w["ex"], ALU.mult)
        if d == "A":
            Vbf = st_hi[:, ob, :].k(ob) if own else w["Vbf"]
            CP("act", Vbf, hi_src)
        else:
            Vbf = st_hi[:, ob, :].k(ob)
        TT("dve", w["Vexp"], Vbf.re("p (h d) -> p h d", h=4).bc(2, [128, 4, 8, 64]),
           cmx.bc(1, [128, 4, 8, 64]), ALU.mult)
        for h in range(4):
            MM(B1[0:64, h * 8:(h + 1) * 8], w["logf"][:, h * 64:(h + 1) * 64], Cm_f)
        ACT(w["a"].re("p h n -> p (h n)"), B1[0:64, 0:32], AF.Exp)
        if own:
            ACT(w["ex"], B2[:, 0:256], AF.Exp, scale=-1.0)
            TT("dve", w["Kbf"], w["kk"], w["ex"], ALU.mult)
            if d == "A":
                ACT(w["sqf"], hq_src, AF.Exp, scale=-1.0)
                ACT(w["sqf"], w["sqf"], AF.Ln, bias=1.0)
                ACT(w["sqf"], w["sqf"], AF.Exp, scale=-1.0)
                TT("dve", w["sqf"], hq_src, w["sqf"], ALU.mult)
                CP("act", st_sq[:, ob, :].k(ob), w["sqf"])
                ACT(w["ex"], B2[:, 0:256], AF.Exp)
                TT("dve", w["Qbf"], w["sqf"], w["ex"], ALU.mult)
            else:
                ACT(w["ex"], B2[:, 0:256], AF.Exp)
                TT("dve", w["Qbf"], st_sq[:, ob, :].k(ob), w["ex"], ALU.mult)
            for h in range(4):
                TR(B3b[0:64, h * 128:(h + 1) * 128], w["Qbf"][:, h * 64:(h + 1) * 64], ident_b)
            for h in range(4):
                TR(B3b[0:64, 512 + h * 128:512 + (h + 1) * 128], w["Kbf"][:, h * 64:(h + 1) * 64], ident_b)
            CP("act", w["QT"][0:64].re("p h t -> p (h t)"), B3b[0:64, 0:512])
            CP("dve", w["KT"][0:64].re("p h t -> p (h t)"), B3b[0:64, 512:1024])
            for h in range(4):
                MM(B4[:, h * 128:(h + 1) * 128], w["KT"][:, h, :], w["QT"][:, h, :])
            TT("dve", w["ATm"], B4.re("p (h t) -> p h t", h=4), M_incl.bc(1, [128, 4, 128]), ALU.mult)
        for h in range(4):
            Bu = B5 if h % 2 == 0 else B6
            MM(Bu[0:64, :], w["Ke"][:, h * 64:(h + 1) * 64], w["Vexp"][:, h, :, :].re("p n d -> p (n d)"))
            CP("act", w["Usb"][:, h, :, :].re("p n d -> p (n d)"), Bu[0:64, :])
        return (blk, d, own, sidx, Vbf)

    def hgrn_late(blk, d, own, sidx, Vbf):
        w = W[sidx % 2]
        Sc = Sall[sidx % 2]
        Sn = Sall[(sidx + 1) % 2]
        ob = blk - 16
        order = list(range(8)) if d == "A" else list(range(7, -1, -1))
        for i, n in enumerate(order):
            last = (i == 7)
            if d == "A":
                dst = Sn[:, 0, :, :] if last else Sc[:, n + 1, :, :]
            else:
                dst = Sn[:, 7, :, :] if last else Sc[:, n - 1, :, :]
            TT(SCAN_ENG, w["tmp"], Sc[:, n, :, :], w["a"][:, :, n].bc(2, [64, 4, 64]), ALU.mult)
            TT(SCAN_ENG, dst, w["tmp"], w["Usb"][:, :, n, :], ALU.add)
        if own:
            CP("act", w["Sb"][0:64], Sc)
            for h in range(4):
                MM(B7[0:64, h * 128:(h + 1) * 128], Vbf[:, h * 64:(h + 1) * 64], w["ATm"][:, h, :],
                   start=True, stop=False, skip=True)
                for n in range(8):
                    MM(B7[0:64, h * 128 + n * 16:h * 128 + (n + 1) * 16], w["Sb"][:, n, h, :],
                       w["QT"][:, h, n * 16:(n + 1) * 16], start=False, stop=(n == 7), skip=True)
            dsto = oacc[:, :, ob * 128:(ob + 1) * 128].k(ob)
            if d == "A":
                CP("act", dsto, B7[0:64, :].re("p (h t) -> p h t", h=4))
            else:
                TT("dve", dsto, dsto, B7[0:64, :].re("p (h t) -> p h t", h=4), ALU.add)

    step = 0
    H_NT = int(os.environ.get("H_NT", "8"))
    H_LVL = int(os.environ.get("H_LVL", "99"))
    pend = [None]
    for tt in range(2 * H_NT):
        xb = xTb[tt % 2]
        DMA("pool", xb, V(xT.ap[:, tt * 256:(tt + 1) * 256].rearrange("(c p) t -> p c t", p=128), []))
        own = tt >= 8
        for j in range(2):
            blk = tt * 2 + j
            ob = blk - 16
            wcur = W[step % 2]
            for c in range(8):
                MM(B0, xb[:, c, j * 128:(j + 1) * 128], wH[:, c, 0:512], start=(c == 0), stop=(c == 7))
            if own:
                for c in range(8):
                    MM(B1, xb[:, c, j * 128:(j + 1) * 128], wH[:, c, 512:1024], start=(c == 0), stop=(c == 7))
                CP("act", st_hfB[:, ob, :].k(ob), B1[:, 256:512])
                CP("act", wcur["hq"], B1[:, 0:256])
            info = hgrn_early(blk, "A", own, B0[:, 0:256], (wcur["hq"] if own else None), B0[:, 256:512], step)
            if pend[0] is not None:
                hgrn_late(*pend[0])
            pend[0] = info
            step += 1
        if own and H_LVL >= 9:
            for h in range(4):
                for c in range(8):
                    MM(B1[0:64, 0:256], wH[:, c, 1024 + h * 64:1024 + (h + 1) * 64], xb[:, c, :],
                       start=(c == 0), stop=(c == 7))
                ACT(geg, B1[0:64, 0:256], AF.Exp, scale=-1.0)
                ACT(geg, geg, AF.Ln, bias=1.0)
                ACT(gateT[:, h, (tt - 8) * 256:(tt - 7) * 256].k(tt, h), geg, AF.Exp, scale=-1.0)
    if pend[0] is not None:
        hgrn_late(*pend[0])
        pend[0] = None
    MS("dve", Sall[step % 2][:, 7, :, :], 0.0)
    for blk in (range(31, 15, -1) if H_LVL >= 50 else []):
        ob = blk - 16
        info = hgrn_early(blk, "B", True, st_hfB[:, ob, :].k(ob), None, None, step)
        if pend[0] is not None:
            hgrn_late(*pend[0])
        pend[0] = info
        step += 1
    if pend[0] is not None:
        hgrn_late(*pend[0])
        pend[0] = None
    fin_sq2 = [fin_sq, V(A.alloc("fin_sq_b", [64, 512], BF16).ap, ["fin_sq_b"])]
    fin_ln2 = [fin_ln, V(A.alloc("fin_ln_b", [64, 512], F32).ap, ["fin_ln_b"])]
    fin_t2 = [fin_t, V(A.alloc("fin_t_b", [64, 512], F32).ap, ["fin_t_b"])]
    fgroups = [(tq, h) for tq in (range(4) if H_LVL >= 60 else []) for h in range(4)]

    def fin_src(g):
        tq, h = fgroups[g]
        sl = slice(tq * 512, (tq + 1) * 512)
        return tq, h, sl, V(oacc.ap[:, h, sl], [("oacc", tq * 4 + i) for i in range(4)])

    def fin_a(g):
        tq, h, sl, osrc = fin_src(g)
        TT("dve", fin_sq2[g % 2], osrc, osrc, ALU.mult)
        MM((B1 if g % 2 == 0 else B2)[0:64, :], ones_b[0:64, 0:64], fin_sq2[g % 2])

    def fin_b(g):
        tq, h, sl, osrc = fin_src(g)
        bk = B1 if g % 2 == 0 else B2
        ACT(fin_ln2[g % 2], bk[0:64, :], AF.Ln, bias=eps_t[0:64, :], scale=1.0 / 64)
        ACT(fin_ln2[g % 2], fin_ln2[g % 2], AF.Exp, scale=-0.5)
        STT("dve", fin_t2[g % 2], osrc, hnw_t[:, h:h + 1], fin_ln2[g % 2], ALU.mult, ALU.mult)
        TT("dve", ohg[0:64, h, sl].k(tq, h), fin_t2[g % 2],
           V(gateT.ap[:, h, sl], [("gateT", 8 + 2 * tq, h), ("gateT", 9 + 2 * tq, h)]), ALU.mult)

    if fgroups:
        fin_a(0)
    for g in range(len(fgroups)):
        if g + 1 < len(fgroups):
            fin_a(g + 1)
        fin_b(g)

    if dbg and stage == 1:
        S.barrier()
        DMA("sp", dbg_o[0:64, 0:8192], oacc.re("p h t -> p (h t)"), final=True)
        DMA("pool", dbg_o[64:128, 0:8192], ohg[0:64].re("p h t -> p (h t)"), final=True)
        MS("dve", fin_t, 0.0)
        S.emit()
        return nc


    S.barrier()
    A.release(pmark)
    SLOPES = [2.0 ** (-8.0 * (h + 1) / 4) for h in range(4)]
    ALIBI_THR = float(os.environ.get("ALIBI_THR", "40"))
    catD = A.alloc("catD", [128, 4, 2048], BF16)
    catM = A.alloc("catM", [128, 4, 2048], BF16)
    a4mark = A.mark()
    MS("dve", catM[64:128], 0.0)
    KTc = A.alloc("KTc", [128, 4, 4096], BF16)
    Vt = A.alloc("Vt", [128, 32, 512], BF16)
    QTp = [A.alloc(f"QTp{m}", [128, 4, 2048], BF16) for m in range(2)]
    mqT = A.alloc("mqT", [128, 2, 2048], BF16)
    mkT = A.alloc("mkT", [128, 2, 256], BF16)
    mvt = A.alloc("mvt", [128, 2, 256], BF16)
    lam_t = A.alloc("lam_t", [128, 8], F32)
    dnw_t = A.alloc("dnw_t", [128, 1], F32)
    amark = A.mark()
    wmem = A.alloc("wmem", [128, 8, 512], BF16)
    memTb = A.alloc("memTb", [128, 8, 256], BF16)
    lamraw = A.alloc("lamraw", [128, 4, 64], F32)
    lamtmp = A.alloc("lamtmp", [128, 2, 64], F32)
    DMA("pool", wmem, V(w_mem.ap.rearrange("(c p) n -> p c n", p=128), []))
    DMA("pool", memTb, V(memT.ap.rearrange("(c p) n -> p c n", p=128), []))
    DMA("sp", lamraw, V(lamv.ap.unsqueeze(0).broadcast_to([128, 4, 64]), []))
    DMA("sp", dnw_t, dnw)
    TT("dve", lamtmp[:, 0, :], lamraw[:, 0, :], lamraw[:, 1, :], ALU.mult)
    TT("dve", lamtmp[:, 1, :], lamraw[:, 2, :], lamraw[:, 3, :], ALU.mult)
    S.op("dve", lambda e: e.reduce_sum(out=lam_t.ap[:, 1:2], in_=lamtmp.ap[:, 0, :], axis=AX.X), lamtmp.keys, lam_t.keys)
    S.op("dve", lambda e: e.reduce_sum(out=lam_t.ap[:, 2:3], in_=lamtmp.ap[:, 1, :], axis=AX.X), lamtmp.keys, lam_t.keys)
    ACT(lam_t[:, 1:3], lam_t[:, 1:3], AF.Exp)
    TT("dve", lam_t[:, 0:1], lam_t[:, 2:3], lam_t[:, 1:2], ALU.subtract)
    TS("dve", lam_t[:, 0:1], lam_t[:, 0:1], -0.2, ALU.add)
    TS("dve", dnw_t, dnw_t, 0.8, ALU.mult)
    for cc in range(2):
        for c in range(8):
            MM(B0[:, 0:256], wmem[:, c, cc * 128:(cc + 1) * 128], memTb[:, c, :], start=(c == 0), stop=(c == 7))
        CP("act", mkT[:, cc, :], B0[:, 0:256])
    for mb in range(2):
        for c in range(8):
            MM(B1[:, 0:256], memTb[:, c, mb * 128:(mb + 1) * 128], wmem[:, c, 256:512], start=(c == 0), stop=(c == 7))
        CP("act", mvt[:, mb, :], B1[:, 0:256])

    S.barrier()
    A.release(amark)
    wA = A.alloc("wA", [128, 8, 1792], BF16)
    xTa = [A.alloc(f"xTa{i}", [128, 8, 512], BF16) for i in range(2)]
    for c in range(8):
        DMA("pool", wA[:, c, :], w_a[c * 128:(c + 1) * 128, :])

    fm_banks = [B0, B1, B2, B3]
    tm_banks = [B4, B5, B6, B7]
    fmi = 0
    tmi = 0
    for tt in range(8):
        xb = xTa[tt % 2]
        DMA("pool", xb, V(xT.ap[:, tt * 512:(tt + 1) * 512].rearrange("(c p) t -> p c t", p=128), []))
        own = tt >= 4
        t0 = tt * 512
        groups = [("k", hh, 512 + hh * 128) for hh in range(4)]
        if own:
            groups += [("q", hh, hh * 128) for hh in range(4)] + [("m", cc, 1024 + cc * 128) for cc in range(2)]
        for kind, idx, col in groups:
            bk = fm_banks[fmi % 4]
            fmi += 1
            for c in range(8):
                MM(bk, wA[:, c, col:col + 128], xb[:, c, :], start=(c == 0), stop=(c == 7))
            if kind == "k":
                CP("act", KTc[:, idx, t0:t0 + 512].k(tt, idx), bk)
            elif kind == "q":
                qs = slice(t0 - 2048, t0 - 2048 + 512)
                CP("act", V(QTp[0].ap[0:64, idx, qs], [("QTp0", tt, idx)]), bk[0:64, :])
                CP("act", V(QTp[1].ap[64:128, idx, qs], [("QTp1", tt, idx)]), bk[64:128, :])
                MS("dve", V(QTp[0].ap[64:128, idx, qs], [("QTp0", tt, idx)]), 0.0)
                MS("dve", V(QTp[1].ap[0:64, idx, qs], [("QTp1", tt, idx)]), 0.0)
            else:
                CP("act", mqT[:, idx, t0 - 2048:t0 - 2048 + 512].k(tt, idx), bk)
        for j in range(4):
            bk = tm_banks[tmi % 4]
            tmi += 1
            for c in range(8):
                MM(bk, xb[:, c, j * 128:(j + 1) * 128], wA[:, c, 1280:1792], start=(c == 0), stop=(c == 7))
            CP("dve", Vt[:, tt * 4 + j, :].k(tt * 4 + j), bk)
    if H_LVL >= 70:
        S.barrier()
        A.release(amark)
        Dst = A.alloc("Dst", [128, 896], F32)
        Pt = [A.alloc(f"Pt{i}", [128, 512], BF16) for i in range(3)]
        sc = [A.alloc(f"sc{i}", [128, 512], F32) for i in range(2)]
        On = [A.alloc(f"On{i}", [128, 512], F32) for i in range(2)]
        Rz = A.alloc("Rz", [128, 512], F32)
        dsq = A.alloc("dsq", [128, 512], BF16)
        rstd = A.alloc("rstd", [128, 512], F32)
        DMA("sp", Dst, dstrip[:, 1536:2432])
        qaug = A.alloc("qaug", [128, 4, 512], BF16)
        sgn = A.alloc("sgn", [128, 256], BF16)
        btab = A.alloc("btab", [128, 4, 2, 32], F32)
        DMA("pool", qaug.re("p h i -> p (h i)"), qaug_d)
        DMA("pool", sgn, sgn_d)
        DMA("sp", btab.re("p h s d -> p (h s d)"), btab_d)
        Pt.append(A.alloc("Pt3", [128, 512], BF16))
        Pt.append(A.alloc("Pt4", [128, 512], BF16))
        Sbanks = [B0, B1, B7, B6]
        tiles = []
        oz = 0
        for h in range(4):
            for qt in range(4):
                q0 = 2048 + qt * 512
                for m in range(2):
                    Ob, Zb = (B2, B3) if oz % 2 == 0 else (B4, B5)
                    oz += 1
                    kbs = []
                    for kb in range(32):
                        k0 = kb * 128
                        if k0 + 127 < q0:
                            md = q0 - k0 - 127
                        elif k0 > q0 + 511:
                            md = k0 - q0 - 511
                        else:
                            md = 0
                        if SLOPES[h] * md <= ALIBI_THR:
                            kbs.append(kb)
                    for ii, kb in enumerate(kbs):
                        tiles.append(dict(h=h, qt=qt, m=m, kb=kb, first=(ii == 0), last=(ii == len(kbs) - 1),
                                          Ob=Ob, Zb=Zb))
        LOOK = 3

        def emit_front(i):
            t = tiles[i]
            h, qt, m, kb = t["h"], t["qt"], t["m"], t["kb"]
            k0 = kb * 128
            q0 = 2048 + qt * 512
            Sb_ = Sbanks[i % 4]
            lhs = V(KTc.ap[:, h, k0:k0 + 128], [("KTc", kb // 4, h)])
            rhs = V(QTp[m].ap[:, h, qt * 512:(qt + 1) * 512], [(f"QTp{m}", qt + 4, h)])
            if k0 + 128 <= q0 or k0 >= q0 + 512:
                left = k0 + 128 <= q0
                dist = (q0 - k0) // 128 if left else (k0 - q0) // 128
                MM(Sb_, lhs, rhs, start=True, stop=False)
                MM(Sb_, sgn[:, 0:128] if left else sgn[:, 128:256], qaug[:, h, :], start=False, stop=True)
                ACT(Pt[i % 5], Sb_, AF.Exp, bias=btab[:, h, 0 if left else 1, dist:dist + 1], scale=0.125)
            else:
                MM(Sb_, lhs, rhs)
                s0 = q0 - k0 + 1920 - 1536
                STT("dve", sc[i % 2], Dst[:, s0:s0 + 512], -8.0 * SLOPES[h], Sb_, ALU.mult, ALU.add)
                ACT(Pt[i % 5], sc[i % 2], AF.Exp, scale=0.125)

        def emit_back(i):
            t = tiles[i]
            h, qt, m, kb = t["h"], t["qt"], t["m"], t["kb"]
            Ob, Zb = t["Ob"], t["Zb"]
            pt = Pt[i % 5]
            MM(Ob, Vt[:, kb, h * 128:(h + 1) * 128].k(kb), pt, start=t["first"], stop=t["last"])
            MM(Zb, ones_b, pt, start=t["first"], stop=t["last"])
            if t["last"]:
                RECIP(Rz, Zb)
                TT("dve", On[m], Ob, Rz, ALU.mult)
                if m == 1:
                    STT("dve", On[0], On[1], lam_t[:, 0:1], On[0], ALU.mult, ALU.add)
                    TT("dve", dsq, On[0], On[0], ALU.mult)
                    deferred.append((i + 3, h, qt))

        deferred = []

        def run_deferred(i, force=False):
            while deferred and (force or deferred[0][0] <= i):
                _, h, qt = deferred.pop(0)
                MM(B6, ones_b, dsq)
                ACT(rstd, B6, AF.Ln, bias=eps_t, scale=1.0 / 128)
                ACT(rstd, rstd, AF.Exp, scale=-0.5)
                STT("dve", catD[:, h, qt * 512:(qt + 1) * 512].k(qt, h), On[0], dnw_t[:, 0:1], rstd,
                    ALU.mult, ALU.mult)

        for i in range(len(tiles) + LOOK):
            if i < len(tiles):
                emit_front(i)
            if i - LOOK >= 0:
                emit_back(i - LOOK)
                run_deferred(i - LOOK)
        run_deferred(0, force=True)
        si = 0
        pi = 0
        xg = 0
        for qt in range(4):
            for h in range(4):
                pr = (h % 2) * 64
                Ob, Zb = (B2, B3) if xg % 2 == 0 else (B4, B5)
                Rx = Rz if xg % 2 == 0 else rstd
                xg += 1
                for mb in range(2):
                    Sb_ = (B0, B1, B7, B6)[si % 4]
                    si += 1
                    pt = Pt[pi % 5]
                    pi += 1
                    MM(Sb_, mkT[pr:pr + 64, h // 2, mb * 128:(mb + 1) * 128],
                       V(mqT.ap[pr:pr + 64, h // 2, qt * 512:(qt + 1) * 512], [("mqT", qt + 4, h // 2)]))
                    ACT(pt, Sb_, AF.Exp, scale=0.125)
                    MM(Ob[0:64, :], mvt[:, mb, h * 64:(h + 1) * 64], pt, start=(mb == 0), stop=(mb == 1))
                    MM(Zb[0:64, :], ones_b[:, 0:64], pt, start=(mb == 0), stop=(mb == 1))
                RECIP(Rx[0:64, :], Zb[0:64, :])
                TT("dve", catM[0:64, h, qt * 512:(qt + 1) * 512].k(qt, h), Ob[0:64, :], Rx[0:64, :], ALU.mult)

    if dbg and stage == 2:
        S.barrier()
        DMA("pool", dbg_o[:, 0:8192], catD.re("p h t -> p (h t)"), final=True)
        DMA("pool", dbg_o[0:64, 8192:16384], catM[0:64].re("p h t -> p (h t)"), final=True)
        S.emit()
        return nc

    S.barrier()
    A.release(a4mark)
    woD = A.alloc("woD", [128, 4, 1024], BF16)
    woH = A.alloc("woH", [128, 8, 1024], BF16)
    lnt = A.alloc("lnt", [128, 4, 1024], F32)
    rw = A.alloc("rw", [128, 8, 32], F32)
    rb = A.alloc("rb", [128, 32], F32)
    carry = A.alloc("carry", [128, 32], F32)
    xt = [A.alloc(f"xt{i}", [128, 1024], F32) for i in range(2)]
    zt = [A.alloc(f"zt{i}", [128, 1024], F32) for i in range(2)]
    hb = [A.alloc(f"hb{i}", [128, 1024], BF16) for i in range(2)]
    def a4set(i):
        d = {}
        d["hT"] = A.alloc(f"hT{i}", [128, 8, 128], F32)
        d["bst"] = A.alloc(f"bst{i}", [128, 2, 6], F32)
        d["mv_"] = A.alloc(f"mv_{i}", [128, 4], F32)
        for nm, w in (("lg", 32), ("v8", 8), ("negv0", 1), ("e4", 4), ("s4", 2), ("msk", 32), ("eg", 32),
                      ("rnk", 32), ("slm", 32), ("ovf", 32), ("oh", 32), ("slf", 4)):
            d[nm] = A.alloc(f"{nm}{i}", [128, w], F32)
        d["mskb"] = A.alloc(f"mskb{i}", [128, 32], BF16)
        return d
    a4 = [a4set(0), a4set(1)]
    bst = a4[0]["bst"]
    mv_ = a4[0]["mv_"]
    ecap = cst_f[:, 904:936]
    tsf = A.alloc("tsf", [128, 16, 4], F32) if dbg else None

    for hh in range(4):
        DMA("pool", woD[:, hh, :], w_o[hh * 128:(hh + 1) * 128, :])
    for i in range(8):
        DMA("pool", woH[0:64, i, :].k(i), w_o[512 + i * 64:512 + (i + 1) * 64, :])
        MS("dve", woH[64:128, i, :].k(i), 0.0)
    DMA("sp", lnt, V(lnp.ap.unsqueeze(0).broadcast_to([128, 4, 1024]), []))
    DMA("sp", rw, V(router_w.ap.rearrange("(c p) e -> p c e", p=128), []))
    DMA("sp", rb, V(router_b.ap.broadcast_to([128, 32]), []))
    MS("dve", carry, 0.0)
    zpad = A.alloc("zpad", [128, 3, 1024], BF16)
    MS("pool", zpad, 0.0)
    for e_ in range(NEXP):
        DMA("sp" if e_ % 2 == 0 else "act",
            V(Xs.ap[e_ * CAP:(e_ + 1) * CAP, :].rearrange("(r p) d -> p r d", p=128), [("Xs", "z", e_)]), zpad)
    xs_ready = A.alloc("xs_ready", [128, 8], F32)
    S.op("dve", lambda e: e.memset(xs_ready.ap, 0.0), [("Xs", "z", e_) for e_ in range(NEXP)], ["Xs_ready"])

    def layer_norm(dst, src, gi, bst=None, mv_=None):
        bst = bst if bst is not None else ln_scratch[0]
        mv_ = mv_ if mv_ is not None else ln_scratch[1]
        for c2 in range(2):
            S.op("dve", lambda e, c2=c2, bst=bst, src=src: e.bn_stats(out=bst.ap[:, c2, :],
                                                                      in_=src.ap[:, c2 * 512:(c2 + 1) * 512]),
                 src.keys, bst.keys)
        S.op("dve", lambda e, bst=bst, mv_=mv_: e.bn_aggr(out=mv_.ap[:, 0:2], in_=bst.ap), bst.keys, mv_.keys)
        ACT(mv_[:, 2:3], mv_[:, 1:2], AF.Ln, bias=eps_t, scale=1.0)
        ACT(mv_[:, 2:3], mv_[:, 2:3], AF.Exp, scale=-0.5)
        TS("dve", dst, src, mv_[:, 0:1], ALU.subtract, mv_[:, 2:3], ALU.mult)
        TT("dve", dst, dst, lnt[:, gi, :], ALU.mult)
        TT("dve", dst, dst, lnt[:, gi + 1, :], ALU.add)

    ln_scratch = [bst, mv_]
    NT = int(os.environ.get("A4_NT", "16"))
    for ti in range(NT):
        t0 = ti * 128
        P_ = a4[ti % 2]
        hT, lg, v8, negv0, e4, s4, msk, mskb, eg, rnk, slm, ovf, oh, slf = (
            P_["hT"], P_["lg"], P_["v8"], P_["negv0"], P_["e4"], P_["s4"], P_["msk"], P_["mskb"], P_["eg"],
            P_["rnk"], P_["slm"], P_["ovf"], P_["oh"], P_["slf"])
        x_t = xt[ti % 2]
        z_t = zt[ti % 2]
        h_b = hb[ti % 2]
        DMA("sp", x_t, x_own[t0:t0 + 128, :])
        for half in range(2):
            bk = (B0, B1)[half] if ti % 2 == 0 else (B2, B3)[half]
            cs = slice(half * 512, (half + 1) * 512)
            n = 0
            for hh in range(4):
                MM(bk, V(catD.ap[:, hh, t0:t0 + 128], []), woD[:, hh, cs], start=(n == 0), stop=False)
                n += 1
            for hh in range(4):
                MM(bk, V(ohg.ap[:, hh, t0:t0 + 128], []), woH[:, hh, cs].k(hh), start=False, stop=False)
            for hh in range(4):
                MM(bk, V(catM.ap[:, hh, t0:t0 + 128], []), woH[:, 4 + hh, cs].k(4 + hh), start=False, stop=(hh == 3))
            STT("dve", z_t[:, cs], x_t[:, cs], ALPHA, bk, ALU.mult, ALU.add)
        layer_norm(z_t, z_t, 0, P_["bst"], P_["mv_"])
        DMA("sp", V(Hf.ap[t0:t0 + 128, :], [("Hf", ti)]), z_t)
        CP("act", h_b, z_t)
        for c in range(8):
            TR((B4 if c < 4 else B5)[:, (c % 4) * 128:(c % 4 + 1) * 128], z_t[:, c * 128:(c + 1) * 128], ident_f)
        CP("act", hT[:, 0:4, :].re("p c t -> p (c t)"), B4)
        CP("act", hT[:, 4:8, :].re("p c t -> p (c t)"), B5)
        for c in range(8):
            MM(B6[:, 0:32], hT[:, c, :], rw[:, c, :], start=(c == 0), stop=(c == 7))
        TT("dve", lg, B6[:, 0:32], rb, ALU.add)
        S.op("dve", lambda e, v8=v8, lg=lg: e.max(out=v8.ap, in_=lg.ap), lg.keys, v8.keys)
        TS("dve", negv0, v8[:, 0:1], -1.0, ALU.mult)
        ACT(e4, v8[:, 0:4], AF.Exp, bias=negv0)
        S.op("dve", lambda e, s4=s4, e4=e4: e.reduce_sum(out=s4.ap[:, 0:1], in_=e4.ap, axis=AX.X), e4.keys, s4.keys)
        RECIP(s4[:, 1:2], s4[:, 0:1])
        TS("dve", tokgate[:, ti, :].k(ti), e4, s4[:, 1:2], ALU.mult)
        TS("dve", msk, lg, v8[:, 3:4], ALU.is_ge)
        ACT(eg, lg, AF.Exp, bias=negv0)
        STT("dve", tokG[:, ti, :].k(ti), eg, s4[:, 1:2], msk, ALU.mult, ALU.mult)
        CP("dve", mskb, msk)
        MM(B7[:, 0:32], tri_b, mskb)
        MM(B7[:, 32:64], ones_b, mskb)
        TT("dve", rnk, B7[:, 0:32], carry, ALU.add)
        TT("dve", carry, B7[:, 32:64], carry, ALU.add)
        TT("dve", slm, rnk, ecap, ALU.add)
        TS("dve", ovf, rnk, float(CAP), ALU.is_gt, 1.0e6, ALU.mult)
        TT("dve", slm, slm, ovf, ALU.add)
        for k in range(4):
            TS("dve", oh, lg, v8[:, k:k + 1], ALU.is_equal)
            TT("dve", oh, oh, slm, ALU.mult)
            S.op("dve", lambda e, k=k, slf=slf, oh=oh: e.reduce_sum(out=slf.ap[:, k:k + 1], in_=oh.ap, axis=AX.X),
                 oh.keys, slf.keys)
        for k in range(4):
            CP("dve", tokslot[ti][k], slf[:, k:k + 1])
        if dbg:
            CP("dve", tsf[:, ti, :].k(ti), slf)
        for k in range(4):
            S.dma("pool", lambda e, ti=ti, k=k, h_b=h_b: e.indirect_dma_start(
                out=Xs.ap, out_offset=bass.IndirectOffsetOnAxis(ap=tokslot[ti][k].ap, axis=0),
                in_=h_b.ap, in_offset=None, bounds_check=bcheck(e), oob_is_err=False),
                list(h_b.keys) + list(tokslot[ti][k].keys) + ["Xs_ready"], [("Xs", "s", ti, k)])

    if dbg and stage == 3:
        S.barrier()
        for ti in range(NT):
            DMA("sp", dbg_o[:, ti * 64:ti * 64 + 32], tokG[:, ti, :], final=True)
            DMA("sp", dbg_o[:, ti * 64 + 32:ti * 64 + 36], tokgate[:, ti, :], final=True)
            DMA("sp", dbg_o[:, ti * 64 + 36:ti * 64 + 40], tsf[:, ti, :], final=True)
        for ti in range(16):
            DMA("sp", out[ti * 128:(ti + 1) * 128, :], Hf[ti * 128:(ti + 1) * 128, :], final=True)
        S.emit()
        return nc

    S.barrier()
    A.release(pM)
    lnt = A.alloc("lnt2", [128, 4, 1024], F32)
    bst = A.alloc("bst2", [128, 2, 6], F32)
    mv_ = A.alloc("mv2_", [128, 4], F32)
    ln_scratch = [bst, mv_]
    DMA("sp", lnt, V(lnp.ap.unsqueeze(0).broadcast_to([128, 4, 1024]), []))
    wgu = [A.alloc(f"wgu{i}", [128, 8, 2048], BF16) for i in range(2)]
    wdn = [A.alloc(f"wdn{i}", [128, 8, 1024], BF16) for i in range(2)]
    bgu = A.alloc("bgu", [128, NEXP, 2, 8], F32)
    xs = A.alloc("xs", [128, 3, 1024], BF16)
    xsT = A.alloc("xsT", [128, 8, CAP], BF16)
    actT = A.alloc("actT", [128, 8, CAP], BF16)
    gt = [A.alloc(f"gt{i}", [128, CAP], F32) for i in range(2)]
    lt = [A.alloc(f"lt{i}", [128, CAP], F32) for i in range(2)]
    sg = [A.alloc(f"sg{i}", [128, CAP], F32) for i in range(2)]
    yb = [A.alloc(f"yb{i}", [128, 1024], BF16) for i in range(2)]
    DMA("sp", bgu.re("p e j c -> p (e j c)"), b_gu)
    NE = int(os.environ.get("M_NE", str(NEXP)))
    B3b_ = B3.bitcast(BF16)
    B7b_ = B7.bitcast(BF16)
    yi = 0
    def load_expert(ee):
        for c in range(8):
            DMA("pool", wgu[ee % 2][:, c, :].k(c), w_gu[ee, c * 128:(c + 1) * 128, :])
        for c in range(8):
            DMA("pool", wdn[ee % 2][:, c, :].k(c), w_dn[ee, c * 128:(c + 1) * 128, :])

    xs2 = [xs, A.alloc("xs_b", [128, 3, 1024], BF16)]
    xsT2 = [xsT, A.alloc("xsT_b", [128, 8, CAP], BF16)]

    def load_xs(ee):
        DMA("sp", xs2[ee % 2], V(Xs.ap[ee * CAP:(ee + 1) * CAP, :].rearrange("(r p) d -> p r d", p=128), Xs.keys))

    def transpose_xs(ee):
        for r in range(3):
            tb = B3b_ if r % 2 == 0 else B7b_
            for c in range(8):
                TR(tb[:, c * 128:(c + 1) * 128], xs2[ee % 2][:, r, c * 128:(c + 1) * 128], ident_b)
            CP("act" if r % 2 == 0 else "dve", xsT2[ee % 2][:, :, r * 128:(r + 1) * 128],
               tb.re("p (c t) -> p c t", c=8))

    load_expert(0)
    load_xs(0)
    transpose_xs(0)
    for e_ in range(NE):
        wg = wgu[e_ % 2]
        wd = wdn[e_ % 2]
        xsT = xsT2[e_ % 2]
        if e_ + 1 < NE:
            load_expert(e_ + 1)
            load_xs(e_ + 1)
        for fc in range(8):
            Bg = B0 if fc % 2 == 0 else B2
            Bl = B1 if fc % 2 == 0 else B4
            for c in range(8):
                MM(Bg[:, 0:CAP], wg[:, c, fc * 128:(fc + 1) * 128].k(c), xsT[:, c, :], start=(c == 0), stop=(c == 7))
            for c in range(8):
                MM(Bl[:, 0:CAP], wg[:, c, 1024 + fc * 128:1024 + (fc + 1) * 128].k(c), xsT[:, c, :],
                   start=(c == 0), stop=(c == 7))
            g_, l_, s_ = gt[fc % 2], lt[fc % 2], sg[fc % 2]
            TS("dve", g_, Bg[:, 0:CAP], bgu[:, e_, 0, fc:fc + 1], ALU.add, 7.0, ALU.min)
            TS("dve", l_, Bl[:, 0:CAP], bgu[:, e_, 1, fc:fc + 1], ALU.add, 7.0, ALU.min)
            ACT(s_, g_, AF.Sigmoid, scale=1.702)
            TS("dve", l_, l_, -7.0, ALU.max, 1.0, ALU.add)
            TT("dve", l_, l_, g_, ALU.mult)
            TT("dve", actT[:, fc, :], l_, s_, ALU.mult)
        if e_ + 1 < NE:
            transpose_xs(e_ + 1)
        for r in range(3):
            y_ = yb[yi % 2]
            yi += 1
            for half in range(2):
                By = B5 if half == 0 else B6
                for fc in range(8):
                    MM(By, actT[:, fc, r * 128:(r + 1) * 128], wd[:, fc, half * 512:(half + 1) * 512].k(fc),
                       start=(fc == 0), stop=(fc == 7))
                CP("act", y_[:, half * 512:(half + 1) * 512], By)
            DMA("sp", V(Ys.ap[e_ * CAP + r * 128:e_ * CAP + (r + 1) * 128, :], [("Ys", e_, r)]), y_)

    S.barrier()
    cmark = A.mark()
    yk = [A.alloc(f"yk{i}", [128, 4, 1024], BF16) for i in range(2)]
    hf = [A.alloc(f"hf{i}", [128, 1024], F32) for i in range(2)]
    bdn_t = A.alloc("bdn_t", [32, 1024], F32)
    GTs = [A.alloc(f"GT{i}", [32, 128], F32) for i in range(2)]
    DMA("sp", bdn_t, b_dn)

    def comb_a(ti):
        t0 = ti * 128
        yk_ = yk[ti % 2]
        h_ = hf[ti % 2]
        GT = GTs[ti % 2]
        S.op("act", lambda e, yk_=yk_: e.memzero(yk_.ap), (), [(yk_.keys[0], k) for k in range(4)])
        for k in range(4):
            S.dma("pool", lambda e, ti=ti, k=k, yk_=yk_: e.indirect_dma_start(
                out=yk_.ap[:, k, :], out_offset=None,
                in_=Ys.ap, in_offset=bass.IndirectOffsetOnAxis(ap=tokslot[ti][k].ap, axis=0),
                bounds_check=bcheck(e), oob_is_err=False),
                list(tokslot[ti][k].keys), [(yk_.keys[0], k)])
        DMA("sp", h_, V(Hf.ap[t0:t0 + 128, :], [("Hf", ti)]))
        TR(B4[0:32, 0:128], tokG[:, ti, :].k(ti), ident_f)
        CP("act", GT, B4[0:32, 0:128])
        for half in range(2):
            bk = (B0, B1)[half] if ti % 2 == 0 else (B2, B3)[half]
            cs = slice(half * 512, (half + 1) * 512)
            MM(bk, GT, bdn_t[:, cs])

    def comb_b(ti):
        t0 = ti * 128
        yk_ = yk[ti % 2]
        h_ = hf[ti % 2]
        for half in range(2):
            bk = (B0, B1)[half] if ti % 2 == 0 else (B2, B3)[half]
            cs = slice(half * 512, (half + 1) * 512)
            STT("dve", h_[:, cs], h_[:, cs], ALPHA, bk, ALU.mult, ALU.add)
        for k in range(4):
            STT("dve", h_, yk_[:, k, :].k(k), tokgate[:, ti, k:k + 1].k(ti), h_, ALU.mult, ALU.add)
        layer_norm(h_, h_, 2)
        DMA("sp", V(out.ap[t0:t0 + 128, :], [("out", ti)]), h_, final=True)

    comb_a(0)
    for ti in range(16):
        if ti + 1 < 16:
            comb_a(ti + 1)
        comb_b(ti)

    S.emit()
    return nc


def make_consts():
    c = np.zeros((128, 1024), np.float32)
    r = np.arange(128)[:, None]
    cc = np.arange(128)[None, :]
    same = (r // 16) == (cc // 16)
    c[:, 0:128] = (same & (r <= cc))
    c[:, 128:256] = (same & (r >= cc))
    c[:, 256:384] = (same & (r < cc))
    c[:, 384:512] = (same & (r > cc))
    c[:, 512:640] = np.eye(128)
    c[:, 640:768] = (r <= cc)
    c[:, 768:776] = (r // 16) == np.arange(8)[None, :]
    c[:, 776:904] = 1.0
    c[:, 904:936] = (np.arange(NEXP) * CAP - 1)[None, :]
    return c


def make_alibi_tabs():
    slopes = [2.0 ** (-8.0 * (h + 1) / 4) for h in range(4)]
    i = np.arange(512)
    qaug = np.zeros((2, 4, 512), np.float32)
    for h, m in enumerate(slopes):
        qaug[0, h] = -8.0 * m * 16.0 * (i // 16)
        qaug[1, h] = -8.0 * m * (i % 16)
    j = np.arange(128)[:, None]
    dist = np.arange(32)[None, :]
    btab = np.zeros((128, 4, 2, 32), np.float32)
    for h, m in enumerate(slopes):
        btab[:, h, 0, :] = m * j - m * 128.0 * dist
        btab[:, h, 1, :] = -m * j - m * 128.0 * dist
    sgn = np.zeros((128, 256), np.float32)
    sgn[0:2, 0:128] = 1.0
    sgn[0:2, 128:256] = -1.0
    qa = np.zeros((128, 2048), np.float32)
    qa[0:2] = qaug.reshape(2, 2048)
    return qa, btab.reshape(128, 256), sgn


def make_dstrip():
    j = np.arange(128)[:, None]
    x = np.arange(6016)[None, :]
    return np.abs(x - j - 1920).astype(np.float32)


def prep_inputs(inp):
    x = np.asarray(inp["x"], np.float32)
    mem = np.asarray(inp["mem"], np.float32)
    w_in = np.asarray(inp["w_in"], np.float32)[0]
    dq, dk, dv = w_in[:, 0:512], w_in[:, 512:1024], w_in[:, 1024:1536]
    hq, hff, hfb, hi, hg, mq = (w_in[:, 1536 + 256 * i:1536 + 256 * (i + 1)] for i in range(6))
    w_a = np.ascontiguousarray(np.concatenate([dq, dk, mq, dv], axis=1))
    lbf = np.asarray(inp["hgrn_lb_fwd"], np.float32)
    lbb = np.asarray(inp["hgrn_lb_bwd"], np.float32)
    w_gu_full = np.asarray(inp["w_gate_up"], np.float32)[0]
    w_gu = np.ascontiguousarray(np.concatenate([w_gu_full[:, :, 0::2], w_gu_full[:, :, 1::2]], axis=2))
    bgu = np.asarray(inp["b_gate_up"], np.float32)[0]
    bt = bgu.reshape(NEXP, 8, 128, 2).transpose(2, 0, 3, 1)
    b_gu = np.ascontiguousarray(bt.reshape(128, NEXP * 16))
    shared = {
        "lamv": np.stack([inp["lam_q1"][0], inp["lam_k1"][0], inp["lam_q2"][0], inp["lam_k2"][0]]).astype(np.float32),
        "hnw": np.ascontiguousarray(np.asarray(inp["hgrn_norm_w"], np.float32)[0].reshape(4, 64).T),
        "dnw": np.asarray(inp["diff_norm_w"], np.float32)[0].reshape(128, 1).copy(),
        "w_mem": np.asarray(inp["w_mem_kv"], np.float32)[0],
        "w_o": np.asarray(inp["w_o"], np.float32)[0],
        "lnp": np.stack([inp["ln1_g"][0], inp["ln1_b"][0], inp["ln2_g"][0], inp["ln2_b"][0]]).astype(np.float32),
        "router_w": np.asarray(inp["router_w"], np.float32)[0],
        "router_b": np.asarray(inp["router_b"], np.float32)[0].reshape(1, 32).copy(),
        "w_gu": w_gu, "b_gu": b_gu,
        "w_dn": np.asarray(inp["w_down"], np.float32)[0],
        "b_dn": np.asarray(inp["b_down"], np.float32)[0],
        "w_a": w_a,
        "cst": make_consts(),
        "dstrip": make_dstrip(),
        "qaug": make_alibi_tabs()[0], "btab": make_alibi_tabs()[1], "sgn": make_alibi_tabs()[2],
    }
    in_maps = []
    for c in range(NCORES):
        b, hf = c // 2, c % 2
        seq = x[b] if hf == 1 else x[b, ::-1]
        if hf == 1:
            hA, hB, lA, lB = hff, hfb, lbf, lbb
        else:
            hA, hB, lA, lB = hfb, hff, lbb, lbf
        m = dict(shared)
        m["xT"] = np.ascontiguousarray(seq.T)
        m["x_own"] = np.ascontiguousarray(seq[2048:])
        m["memT"] = np.ascontiguousarray(mem[b].T)
        m["w_h"] = np.ascontiguousarray(np.concatenate([hA, hi, hq, hB, hg], axis=1))
        m["lbp"] = np.ascontiguousarray(np.stack([lA[0], lA[1], lB[0], lB[1]]))
        in_maps.append(m)
    return in_maps


_NC_CACHE = {}


def kernel(**inputs):
    in_maps = prep_inputs(inputs)
    if "nc" not in _NC_CACHE:
        _NC_CACHE["nc"] = build_program()
    nc = _NC_CACHE["nc"]
    res = run_bass_kernel_spmd(nc, in_maps, core_ids=list(range(NCORES)))
    out = np.zeros((4, 4096, 1024), np.float32)
    for c in range(NCORES):
        b, hf = c // 2, c % 2
        o = np.asarray(res.results[c]["out"], np.float32)
        if hf == 1:
            out[b, 2048:] = o
        else:
            out[b, 0:2048] = o[::-1]
    return out
```

```python
import os
import numpy as np
import ml_dtypes
import concourse.bass as bass
import concourse.mybir as mybir
from concourse.bass_utils import run_bass_kernel_spmd

F32 = mybir.dt.float32
BF16 = mybir.dt.bfloat16
I32 = mybir.dt.int32
ALU = mybir.AluOpType
AF = mybir.ActivationFunctionType
AX = mybir.AxisListType

ENGS = ("pe", "act", "dve", "pool", "sp")
EPOCH = 8192
NDMASEM = 8
NCORES = 8
CAP = 384
NEXP = 32
ALPHA = 2.0 ** 0.25
EPS = 1e-5


class Sched:
    def __init__(self, nc, same_engine_sync=True):
        self.nc = nc
        self.q = {e: [] for e in ENGS}
        self.count = {e: 0 for e in ENGS}
        self.dcount = {e: 0 for e in ENGS}
        self.last_write = {}
        self.readers = {}
        self.waited_eng = {e: {} for e in ENGS}
        self.waited_dma = {e: {} for e in ENGS}
        self.same = same_engine_sync
        self.sems = {}
        self.final_events = []
        self.pending = {e: [] for e in ENGS}

    def _deps_for(self, eng, reads, writes):
        evs = list(self.pending[eng])
        self.pending[eng] = []
        for k in reads:
            w = self.last_write.get(k)
            if w is not None:
                evs.append(w)
            if isinstance(k, str) and k[0] == "B" and k[1:].isdigit():
                evs.extend(r for r in self.readers.get(k, ()) if r[1] != eng)
        for k in writes:
            w = self.last_write.get(k)
            if w is not None:
                evs.append(w)
            evs.extend(self.readers.get(k, ()))
        return evs

    def _reduce(self, eng, evs):
        waits = []
        best = {}
        for ev in evs:
            if ev[0] == "eng":
                _, e, idx = ev
                if e == eng and (not self.same or e == "pe"):
                    continue
                if self.waited_eng[eng].get(e, -1) >= idx:
                    continue
                if best.get(e, -1) < idx:
                    best[e] = idx
            else:
                _, e, d = ev
                if self.waited_dma[eng].get((e, d % NDMASEM), -1) >= d:
                    continue
                self.waited_dma[eng][(e, d % NDMASEM)] = d
                waits.append((("dma", e, d % NDMASEM), 16 * (d // NDMASEM + 1)))
        for e, idx in best.items():
            self.waited_eng[eng][e] = idx
            waits.append((("eng", e, idx // EPOCH), idx % EPOCH + 1))
        return waits

    def _record(self, ev, reads, writes):
        for k in reads:
            self.readers.setdefault(k, []).append(ev)
        for k in writes:
            self.last_write[k] = ev
            self.readers[k] = []

    def op(self, eng, fn, reads=(), writes=()):
        evs = self._deps_for(eng, reads, writes)
        waits = self._reduce(eng, evs)
        idx = self.count[eng]
        self.count[eng] += 1
        self.q[eng].append((fn, waits, (("eng", eng, idx // EPOCH), 1)))
        ev = ("eng", eng, idx)
        self._record(ev, reads, writes)
        return ev

    def dma(self, eng, fn, reads=(), writes=(), final=False):
        evs = self._deps_for(eng, reads, writes)
        d = self.dcount[eng]
        self.dcount[eng] += 1
        if d >= NDMASEM:
            evs.append(("dma", eng, d - NDMASEM))
        waits = self._reduce(eng, evs)
        self.q[eng].append((fn, waits, (("dma", eng, d % NDMASEM), 16)))
        ev = ("dma", eng, d)
        self._record(ev, reads, writes)
        if final:
            self.final_events.append(ev)
        return ev

    def barrier(self):
        evs = []
        for e in ENGS:
            if self.count[e] > 0:
                evs.append(("eng", e, self.count[e] - 1))
            for d in range(max(0, self.dcount[e] - NDMASEM), self.dcount[e]):
                evs.append(("dma", e, d))
        for e in ENGS:
            self.pending[e].extend(evs)
        self.last_write.clear()
        self.readers.clear()

    def emit(self):
        nc = self.nc
        from contextlib import ExitStack
        keys = set()
        for e in ENGS:
            for fn, waits, inc in self.q[e]:
                keys.add(inc[0])
                for k, v in waits:
                    keys.add(k)
        fin = self._reduce("sp", self.final_events)
        for k, v in fin:
            keys.add(k)
        with ExitStack() as st:
            for k in sorted(keys, key=str):
                self.sems[k] = st.enter_context(nc.semaphore("s_" + "_".join(str(x) for x in k)))
            st.enter_context(nc.allow_low_precision(reason="bf16 matmul operands, fp32 accumulation"))
            block = st.enter_context(nc.Block())
            engmap = {"pe": block.tensor, "act": block.scalar, "dve": block.vector,
                      "pool": block.gpsimd, "sp": block.sync}
            for e in ENGS:
                items = self.q[e]
                extra = fin if e == "sp" else []

                def body(engine, items=items, extra=extra):
                    for fn, waits, inc in items:
                        for k, v in waits:
                            engine.wait_ge(self.sems[k], v)
                        ins = fn(engine)
                        ins.then_inc(self.sems[inc[0]], inc[1])
                    for k, v in extra:
                        engine.wait_ge(self.sems[k], v)
                if items or extra:
                    engmap[e](body)


class V:
    __slots__ = ("ap", "keys")

    def __init__(self, ap, keys):
        self.ap = ap
        self.keys = tuple(keys)

    def __getitem__(self, idx):
        return V(self.ap[idx], self.keys)

    def k(self, *suffix):
        return V(self.ap, [(self.keys[0],) + tuple(suffix)])

    def re(self, s, **kw):
        return V(self.ap.rearrange(s, **kw), self.keys)

    def bc(self, axis, shape):
        return V(self.ap.unsqueeze(axis).broadcast_to(list(shape)), self.keys)

    def bitcast(self, dt):
        return V(self.ap.bitcast(dt), self.keys)


_DTSIZE = {F32: 4, BF16: 2, I32: 4}


class Arena:
    def __init__(self, nc, base=16512, limit=229376 - 256):
        self.nc = nc
        self.off = base
        self.limit = limit
        self.n = 0

    def alloc(self, name, shape, dtype):
        size = int(np.prod(shape[1:])) * _DTSIZE[dtype]
        self.off = (self.off + 63) // 64 * 64
        assert self.off + size <= self.limit, f"SBUF overflow at {name}: {self.off + size}"
        self.n += 1
        t = self.nc.alloc_sbuf_tensor_at(f"{name}_{self.n}", list(shape), dtype, offset=self.off)
        self.off += size
        return V(t.ap(), [name])

    def mark(self):
        return self.off

    def release(self, m):
        self.off = m


def build_program(stage=99, dbg=False):
    nc = bass.Bass("TRN2", target_bir_lowering=False)
    S = Sched(nc)

    def din(name, shape, dt=F32):
        return V(nc.dram_tensor(name, list(shape), dt, kind="ExternalInput").ap(), [])

    xT = din("xT", [1024, 4096])
    x_own = din("x_own", [2048, 1024])
    memT = din("memT", [1024, 256])
    w_h = din("w_h", [1024, 1280])
    w_a = din("w_a", [1024, 1792])
    lbp = din("lbp", [4, 256])
    hnw = din("hnw", [64, 4])
    lamv = din("lamv", [4, 64])
    dnw = din("dnw", [128, 1])
    w_mem = din("w_mem", [1024, 512])
    w_o = din("w_o", [1024, 1024])
    lnp = din("lnp", [4, 1024])
    router_w = din("router_w", [1024, 32])
    router_b = din("router_b", [1, 32])
    NE_D = NEXP if stage >= 3 else 1
    w_gu = din("w_gu", [NE_D, 1024, 2048])
    b_gu = din("b_gu", [128, NEXP * 16])
    w_dn = din("w_dn", [NE_D, 1024, 1024])
    b_dn = din("b_dn", [NEXP, 1024])
    cst = din("cst", [128, 1024])
    dstrip = din("dstrip", [128, 6016])
    qaug_d = din("qaug", [128, 2048])
    btab_d = din("btab", [128, 256])
    sgn_d = din("sgn", [128, 256])
    out = V(nc.dram_tensor("out", [2048, 1024], F32, kind="ExternalOutput").ap(), ["out"])
    dbg_o = None
    if dbg:
        dbg_o = V(nc.dram_tensor("dbg", [128, 16384], F32, kind="ExternalOutput").ap(), ["dbg"])
    Xs = V(nc.dram_tensor("Xs", [NEXP * CAP, 1024], BF16, kind="Internal").ap(), ["Xs"])
    Ys = V(nc.dram_tensor("Ys", [NEXP * CAP, 1024], BF16, kind="Internal").ap(), ["Ys"])
    Hf = V(nc.dram_tensor("Hf", [2048, 1024], F32, kind="Internal").ap(), ["Hf"])

    A = Arena(nc)
    banks = []
    for i in range(8):
        t = nc.alloc_psum_tensor(f"bank{i}", [128, 512], F32)
        banks.append(V(t.ap(), [f"B{i}"]))

    _breg = {}

    def bcheck(e):
        if "r" not in _breg:
            _breg["r"] = e.to_reg(NEXP * CAP - 1)
        return _breg["r"]

    def MM(o, lhsT, rhs, start=True, stop=True, skip=False):
        rd = list(lhsT.keys) + list(rhs.keys) + ([] if start else list(o.keys))
        kw = {"skip_group_check": True} if skip else {}
        S.op("pe", lambda e: e.matmul(o.ap, lhsT=lhsT.ap, rhs=rhs.ap, start=start, stop=stop, **kw), rd, o.keys)

    def TR(o, in_, ident):
        S.op("pe", lambda e: e.transpose(out=o.ap, in_=in_.ap, identity=ident.ap),
             list(in_.keys) + list(ident.keys), o.keys)

    def ACT(o, in_, func, bias=None, scale=1.0):
        rd = list(in_.keys)
        kw = {}
        if isinstance(bias, V):
            rd += list(bias.keys)
            kw["bias"] = bias.ap
        elif bias is not None:
            kw["bias"] = bias
        S.op("act", lambda e: e.activation(out=o.ap, in_=in_.ap, func=func, scale=scale, **kw), rd, o.keys)

    def TT(eng, o, a, b, op):
        S.op(eng, lambda e: e.tensor_tensor(out=o.ap, in0=a.ap, in1=b.ap, op=op),
             list(a.keys) + list(b.keys), o.keys)

    def TS(eng, o, a, s1, op0, s2=None, op1=None):
        rd = list(a.keys)
        v1 = s1
        v2 = s2
        if isinstance(s1, V):
            rd += list(s1.keys)
            v1 = s1.ap
        if isinstance(s2, V):
            rd += list(s2.keys)
            v2 = s2.ap
        if op1 is None:
            S.op(eng, lambda e: e.tensor_scalar(out=o.ap, in0=a.ap, scalar1=v1, scalar2=None, op0=op0), rd, o.keys)
        else:
            S.op(eng, lambda e: e.tensor_scalar(out=o.ap, in0=a.ap, scalar1=v1, scalar2=v2, op0=op0, op1=op1),
                 rd, o.keys)

    def STT(eng, o, in0, scalar, in1, op0, op1):
        rd = list(in0.keys) + list(in1.keys)
        sv = scalar
        if isinstance(scalar, V):
            rd += list(scalar.keys)
            sv = scalar.ap
        S.op(eng, lambda e: e.scalar_tensor_tensor(out=o.ap, in0=in0.ap, scalar=sv, in1=in1.ap, op0=op0, op1=op1),
             rd, o.keys)

    def CP(eng, o, in_):
        if eng == "act":
            ACT(o, in_, AF.Copy)
        else:
            S.op(eng, lambda e: e.tensor_copy(out=o.ap, in_=in_.ap), in_.keys, o.keys)

    def MS(eng, o, val):
        S.op(eng, lambda e: e.memset(o.ap, val), (), o.keys)

    def RECIP(o, in_):
        S.op("dve", lambda e: e.reciprocal(out=o.ap, in_=in_.ap), in_.keys, o.keys)

    def DMA(q, o, in_, final=False):
        S.dma(q, lambda e: e.dma_start(out=o.ap, in_=in_.ap), in_.keys, o.keys, final=final)

    cst_f = A.alloc("cst_f", [128, 1024], F32)
    cst_b = A.alloc("cst_b", [128, 1024], BF16)
    small = A.alloc("small", [128, 64], F32)
    tokslot_t = A.alloc("tokslot", [128, 64], I32)
    tokslot = [[tokslot_t[:, ti * 4 + k:ti * 4 + k + 1].k(ti, k) for k in range(4)] for ti in range(16)]
    tokgate = A.alloc("tokgate", [128, 16, 4], F32)
    tokG = A.alloc("tokG", [128, 16, 32], F32)
    pM = A.mark()
    ohg = A.alloc("ohg", [128, 4, 2048], BF16)

    eps_t = small[:, 0:1].k("eps")
    MS("dve", eps_t, EPS)
    DMA("sp", cst_f, cst)
    DMA("pool", cst_b, cst)
    MA = cst_f[:, 0:128]
    MB = cst_f[:, 128:256]
    MAx = cst_f[:, 256:384]
    MBx = cst_f[:, 384:512]
    ident_f = cst_f[:, 512:640]
    Cm_f = cst_f[:, 768:776]
    ident_b = cst_b[:, 512:640]
    tri_b = cst_b[:, 640:768]
    ones_b = cst_b[:, 776:904]
    cmx_b_src = cst_b[:, 768:776]

    pmark = A.mark()

    wH = A.alloc("wH", [128, 8, 1280], BF16)
    xTb = [A.alloc(f"xTb{i}", [128, 8, 256], BF16) for i in range(2)]
    st_sq = A.alloc("st_sq", [128, 16, 256], BF16)
    st_hi = A.alloc("st_hi", [128, 16, 256], BF16)
    st_hfB = A.alloc("st_hfB", [128, 16, 256], F32)
    gateT = A.alloc("gateT", [64, 4, 2048], BF16)
    oacc = A.alloc("oacc", [64, 4, 2048], F32)
    lbt = A.alloc("lbt", [128, 4, 256], F32)
    hnw_t = A.alloc("hnw_t", [64, 4], F32)
    cmx = A.alloc("cmx", [128, 8, 64], BF16)
    Sall = [A.alloc(f"Sall{i}", [64, 8, 4, 64], F32) for i in range(2)]

    def wset(i):
        d = {}
        for nm in ("f", "logf", "kk", "ex", "sqf", "hq"):
            d[nm] = A.alloc(f"{nm}{i}", [128, 256], F32)
        for nm in ("Ke", "Kbf", "Qbf", "Vbf"):
            d[nm] = A.alloc(f"{nm}{i}", [128, 256], BF16)
        d["Vexp"] = A.alloc(f"Vexp{i}", [128, 4, 8, 64], BF16)
        d["QT"] = A.alloc(f"QT{i}", [128, 4, 128], BF16)
        d["KT"] = A.alloc(f"KT{i}", [128, 4, 128], BF16)
        d["ATm"] = A.alloc(f"ATm{i}", [128, 4, 128], BF16)
        d["a"] = A.alloc(f"a{i}", [64, 4, 8], F32)
        d["Usb"] = A.alloc(f"Usb{i}", [64, 4, 8, 64], F32)
        d["Sb"] = A.alloc(f"Sb{i}", [128, 8, 4, 64], BF16)
        d["tmp"] = A.alloc(f"stmp{i}", [64, 4, 64], F32)
        return d
    W = [wset(0)]
    W1 = dict(W[0])
    W1["QT"] = A.alloc("QT1", [128, 4, 128], BF16)
    W1["ATm"] = A.alloc("ATm1", [128, 4, 128], BF16)
    W1["a"] = A.alloc("a1", [64, 4, 8], F32)
    W1["Usb"] = A.alloc("Usb1", [64, 4, 8, 64], F32)
    W.append(W1)
    lbm = A.mark()
    lbraw = A.alloc("lbraw", [128, 4, 256], F32)
    A.release(lbm)
    _f = A.alloc("fin_sq", [64, 512], BF16)
    fin_sq = V(_f.ap, ["fin_sq", "lbraw"])
    _f = A.alloc("fin_ln", [64, 512], F32)
    fin_ln = V(_f.ap, ["fin_ln", "lbraw"])
    _f = A.alloc("fin_t", [64, 512], F32)
    fin_t = V(_f.ap, ["fin_t", "lbraw"])
    _f = A.alloc("geg", [64, 256], F32)
    geg = V(_f.ap, ["geg", "lbraw"])

    B0, B1, B2, B3, B4, B5, B6, B7 = banks
    B3b = B3.bitcast(BF16)

    for c in range(8):
        DMA("pool", wH[:, c, :], w_h[c * 128:(c + 1) * 128, :])
    DMA("sp", lbraw, V(lbp.ap.unsqueeze(0).broadcast_to([128, 4, 256]), []))
    DMA("sp", hnw_t, hnw)
    CP("pool", cmx, cmx_b_src.bc(2, [128, 8, 64]))
    for di in range(2):
        TT("dve", lbt[:, 2 * di, :], lbraw[:, 2 * di + 1, :], lbraw[:, 2 * di, :], ALU.subtract)
        ACT(lbt[:, 2 * di, :], lbt[:, 2 * di, :], AF.Exp)
        TS("dve", lbt[:, 2 * di, :], lbt[:, 2 * di, :], 1.0, ALU.add)
        RECIP(lbt[:, 2 * di, :], lbt[:, 2 * di, :])
        TS("dve", lbt[:, 2 * di + 1, :], lbt[:, 2 * di, :], -1.0, ALU.mult, 1.0, ALU.add)
    MS("dve", Sall[0], 0.0)
    MS("dve", Sall[1], 0.0)
    for _t in (W[0]["QT"], W[1]["QT"], W[0]["KT"], W[0]["Sb"]):
        MS("dve", _t[64:128], 0.0)
    MS("dve", ohg[64:128], 0.0)

    SCAN_ENG = os.environ.get("SCAN_ENG", "pool")

    def hgrn_early(blk, d, own, hf_src, hq_src, hi_src, sidx):
        w = W[sidx % 2]
        di = 0 if d == "A" else 1
        lb, omlb = lbt[:, 2 * di, :], lbt[:, 2 * di + 1, :]
        M_incl, M_excl = (MA, MBx) if d == "A" else (MB, MAx)
        ob = blk - 16
        ACT(w["f"], hf_src, AF.Exp)
        ACT(w["f"], w["f"], AF.Ln, bias=1.0)
        ACT(w["f"], w["f"], AF.Exp, scale=-1.0)
        TT("dve", w["kk"], w["f"], omlb, ALU.mult)
        ACT(w["logf"], w["kk"], AF.Ln, bias=1.0, scale=-1.0)
        MM(B2[:, 0:256], M_incl, w["logf"])
        MM(B2[:, 256:512], M_excl, w["logf"])
        ACT(w["ex"], B2[:, 256:512], AF.Exp)
        TT("dve", w["Ke"], w["kk"], w["ex"], ALU.mult)
        if d == "A":
            Vbf = st_hi[:, ob, :].k(ob) if own else w["Vbf"]
            CP("act", Vbf, hi_src)
        else:
            Vbf = st_hi[:, ob, :].k(ob)
        TT("dve", w["Vexp"], Vbf.re("p (h d) -> p h d", h=4).bc(2, [128, 4, 8, 64]),
           cmx.bc(1, [128, 4, 8, 64]), ALU.mult)
        for h in range(4):
            MM(B1[0:64, h * 8:(h + 1) * 8], w["logf"][:, h * 64:(h + 1) * 64], Cm_f)
        ACT(w["a"].re("p h n -> p (h n)"), B1[0:64, 0:32], AF.Exp)
        if own:
            ACT(w["ex"], B2[:, 0:256], AF.Exp, scale=-1.0)
            TT("dve", w["Kbf"], w["kk"], w["ex"], ALU.mult)
            if d == "A":
                ACT(w["sqf"], hq_src, AF.Exp, scale=-1.0)
                ACT(w["sqf"], w["sqf"], AF.Ln, bias=1.0)
                ACT(w["sqf"], w["sqf"], AF.Exp, scale=-1.0)
                TT("dve", w["sqf"], hq_src, w["sqf"], ALU.mult)
                CP("act", st_sq[:, ob, :].k(ob), w["sqf"])
                ACT(w["ex"], B2[:, 0:256], AF.Exp)
                TT("dve", w["Qbf"], w["sqf"], w["ex"], ALU.mult)
            else:
                ACT(w["ex"], B2[:, 0:256], AF.Exp)
                TT("dve", w["Qbf"], st_sq[:, ob, :].k(ob), w["ex"], ALU.mult)
            for h in range(4):
                TR(B3b[0:64, h * 128:(h + 1) * 128], w["Qbf"][:, h * 64:(h + 1) * 64], ident_b)
            for h in range(4):
                TR(B3b[0:64, 512 + h * 128:512 + (h + 1) * 128], w["Kbf"][:, h * 64:(h + 1) * 64], ident_b)
            CP("act", w["QT"][0:64].re("p h t -> p (h t)"), B3b[0:64, 0:512])
            CP("dve", w["KT"][0:64].re("p h t -> p (h t)"), B3b[0:64, 512:1024])
            for h in range(4):
                MM(B4[:, h * 128:(h + 1) * 128], w["KT"][:, h, :], w["QT"][:, h, :])
            TT("dve", w["ATm"], B4.re("p (h t) -> p h t", h=4), M_incl.bc(1, [128, 4, 128]), ALU.mult)
        for h in range(4):
            Bu = B5 if h % 2 == 0 else B6
            MM(Bu[0:64, :], w["Ke"][:, h * 64:(h + 1) * 64], w["Vexp"][:, h, :, :].re("p n d -> p (n d)"))
            CP("act", w["Usb"][:, h, :, :].re("p n d -> p (n d)"), Bu[0:64, :])
        return (blk, d, own, sidx, Vbf)

    def hgrn_late(blk, d, own, sidx, Vbf):
        w = W[sidx % 2]
        Sc = Sall[sidx % 2]
        Sn = Sall[(sidx + 1) % 2]
        ob = blk - 16
        order = list(range(8)) if d == "A" else list(range(7, -1, -1))
        for i, n in enumerate(order):
            last = (i == 7)
            if d == "A":
                dst = Sn[:, 0, :, :] if last else Sc[:, n + 1, :, :]
            else:
                dst = Sn[:, 7, :, :] if last else Sc[:, n - 1, :, :]
            TT(SCAN_ENG, w["tmp"], Sc[:, n, :, :], w["a"][:, :, n].bc(2, [64, 4, 64]), ALU.mult)
            TT(SCAN_ENG, dst, w["tmp"], w["Usb"][:, :, n, :], ALU.add)
        if own:
            CP("act", w["Sb"][0:64], Sc)
            for h in range(4):
                MM(B7[0:64, h * 128:(h + 1) * 128], Vbf[:, h * 64:(h + 1) * 64], w["ATm"][:, h, :],
                   start=True, stop=False, skip=True)
                for n in range(8):
                    MM(B7[0:64, h * 128 + n * 16:h * 128 + (n + 1) * 16], w["Sb"][:, n, h, :],
                       w["QT"][:, h, n * 16:(n + 1) * 16], start=False, stop=(n == 7), skip=True)
            dsto = oacc[:, :, ob * 128:(ob + 1) * 128].k(ob)
            if d == "A":
                CP("act", dsto, B7[0:64, :].re("p (h t) -> p h t", h=4))
            else:
                TT("dve", dsto, dsto, B7[0:64, :].re("p (h t) -> p h t", h=4), ALU.add)

    step = 0
    H_NT = int(os.environ.get("H_NT", "8"))
    H_LVL = int(os.environ.get("H_LVL", "99"))
    pend = [None]
    for tt in range(2 * H_NT):
        xb = xTb[tt % 2]
        DMA("pool", xb, V(xT.ap[:, tt * 256:(tt + 1) * 256].rearrange("(c p) t -> p c t", p=128), []))
        own = tt >= 8
        for j in range(2):
            blk = tt * 2 + j
            ob = blk - 16
            wcur = W[step % 2]
            for c in range(8):
                MM(B0, xb[:, c, j * 128:(j + 1) * 128], wH[:, c, 0:512], start=(c == 0), stop=(c == 7))
            if own:
                for c in range(8):
                    MM(B1, xb[:, c, j * 128:(j + 1) * 128], wH[:, c, 512:1024], start=(c == 0), stop=(c == 7))
                CP("act", st_hfB[:, ob, :].k(ob), B1[:, 256:512])
                CP("act", wcur["hq"], B1[:, 0:256])
            info = hgrn_early(blk, "A", own, B0[:, 0:256], (wcur["hq"] if own else None), B0[:, 256:512], step)
            if pend[0] is not None:
                hgrn_late(*pend[0])
            pend[0] = info
            step += 1
        if own and H_LVL >= 9:
            for h in range(4):
                for c in range(8):
                    MM(B1[0:64, 0:256], wH[:, c, 1024 + h * 64:1024 + (h + 1) * 64], xb[:, c, :],
                       start=(c == 0), stop=(c == 7))
                ACT(geg, B1[0:64, 0:256], AF.Exp, scale=-1.0)
                ACT(geg, geg, AF.Ln, bias=1.0)
                ACT(gateT[:, h, (tt - 8) * 256:(tt - 7) * 256].k(tt, h), geg, AF.Exp, scale=-1.0)
    if pend[0] is not None:
        hgrn_late(*pend[0])
        pend[0] = None
    MS("dve", Sall[step % 2][:, 7, :, :], 0.0)
    for blk in (range(31, 15, -1) if H_LVL >= 50 else []):
        ob = blk - 16
        info = hgrn_early(blk, "B", True, st_hfB[:, ob, :].k(ob), None, None, step)
        if pend[0] is not None:
            hgrn_late(*pend[0])
        pend[0] = info
        step += 1
    if pend[0] is not None:
        hgrn_late(*pend[0])
        pend[0] = None
    fin_sq2 = [fin_sq, V(A.alloc("fin_sq_b", [64, 512], BF16).ap, ["fin_sq_b"])]
    fin_ln2 = [fin_ln, V(A.alloc("fin_ln_b", [64, 512], F32).ap, ["fin_ln_b"])]
    fin_t2 = [fin_t, V(A.alloc("fin_t_b", [64, 512], F32).ap, ["fin_t_b"])]
    fgroups = [(tq, h) for tq in (range(4) if H_LVL >= 60 else []) for h in range(4)]

    def fin_src(g):
        tq, h = fgroups[g]
        sl = slice(tq * 512, (tq + 1) * 512)
        return tq, h, sl, V(oacc.ap[:, h, sl], [("oacc", tq * 4 + i) for i in range(4)])

    def fin_a(g):
        tq, h, sl, osrc = fin_src(g)
        TT("dve", fin_sq2[g % 2], osrc, osrc, ALU.mult)
        MM((B1 if g % 2 == 0 else B2)[0:64, :], ones_b[0:64, 0:64], fin_sq2[g % 2])

    def fin_b(g):
        tq, h, sl, osrc = fin_src(g)
        bk = B1 if g % 2 == 0 else B2
        ACT(fin_ln2[g % 2], bk[0:64, :], AF.Ln, bias=eps_t[0:64, :], scale=1.0 / 64)
        ACT(fin_ln2[g % 2], fin_ln2[g % 2], AF.Exp, scale=-0.5)
        STT("dve", fin_t2[g % 2], osrc, hnw_t[:, h:h + 1], fin_ln2[g % 2], ALU.mult, ALU.mult)
        TT("dve", ohg[0:64, h, sl].k(tq, h), fin_t2[g % 2],
           V(gateT.ap[:, h, sl], [("gateT", 8 + 2 * tq, h), ("gateT", 9 + 2 * tq, h)]), ALU.mult)

    if fgroups:
        fin_a(0)
    for g in range(len(fgroups)):
        if g + 1 < len(fgroups):
            fin_a(g + 1)
        fin_b(g)

    if dbg and stage == 1:
        S.barrier()
        DMA("sp", dbg_o[0:64, 0:8192], oacc.re("p h t -> p (h t)"), final=True)
        DMA("pool", dbg_o[64:128, 0:8192], ohg[0:64].re("p h t -> p (h t)"), final=True)
        MS("dve", fin_t, 0.0)
        S.emit()
        return nc


    S.barrier()
    A.release(pmark)
    SLOPES = [2.0 ** (-8.0 * (h + 1) / 4) for h in range(4)]
    ALIBI_THR = float(os.environ.get("ALIBI_THR", "40"))
    catD = A.alloc("catD", [128, 4, 2048], BF16)
    catM = A.alloc("catM", [128, 4, 2048], BF16)
    a4mark = A.mark()
    MS("dve", catM[64:128], 0.0)
    KTc = A.alloc("KTc", [128, 4, 4096], BF16)
    Vt = A.alloc("Vt", [128, 32, 512], BF16)
    QTp = [A.alloc(f"QTp{m}", [128, 4, 2048], BF16) for m in range(2)]
    mqT = A.alloc("mqT", [128, 2, 2048], BF16)
    mkT = A.alloc("mkT", [128, 2, 256], BF16)
    mvt = A.alloc("mvt", [128, 2, 256], BF16)
    lam_t = A.alloc("lam_t", [128, 8], F32)
    dnw_t = A.alloc("dnw_t", [128, 1], F32)
    amark = A.mark()
    wmem = A.alloc("wmem", [128, 8, 512], BF16)
    memTb = A.alloc("memTb", [128, 8, 256], BF16)
    lamraw = A.alloc("lamraw", [128, 4, 64], F32)
    lamtmp = A.alloc("lamtmp", [128, 2, 64], F32)
    DMA("pool", wmem, V(w_mem.ap.rearrange("(c p) n -> p c n", p=128), []))
    DMA("pool", memTb, V(memT.ap.rearrange("(c p) n -> p c n", p=128), []))
    DMA("sp", lamraw, V(lamv.ap.unsqueeze(0).broadcast_to([128, 4, 64]), []))
    DMA("sp", dnw_t, dnw)
    TT("dve", lamtmp[:, 0, :], lamraw[:, 0, :], lamraw[:, 1, :], ALU.mult)
    TT("dve", lamtmp[:, 1, :], lamraw[:, 2, :], lamraw[:, 3, :], ALU.mult)
    S.op("dve", lambda e: e.reduce_sum(out=lam_t.ap[:, 1:2], in_=lamtmp.ap[:, 0, :], axis=AX.X), lamtmp.keys, lam_t.keys)
    S.op("dve", lambda e: e.reduce_sum(out=lam_t.ap[:, 2:3], in_=lamtmp.ap[:, 1, :], axis=AX.X), lamtmp.keys, lam_t.keys)
    ACT(lam_t[:, 1:3], lam_t[:, 1:3], AF.Exp)
    TT("dve", lam_t[:, 0:1], lam_t[:, 2:3], lam_t[:, 1:2], ALU.subtract)
    TS("dve", lam_t[:, 0:1], lam_t[:, 0:1], -0.2, ALU.add)
    TS("dve", dnw_t, dnw_t, 0.8, ALU.mult)
    for cc in range(2):
        for c in range(8):
            MM(B0[:, 0:256], wmem[:, c, cc * 128:(cc + 1) * 128], memTb[:, c, :], start=(c == 0), stop=(c == 7))
        CP("act", mkT[:, cc, :], B0[:, 0:256])
    for mb in range(2):
        for c in range(8):
            MM(B1[:, 0:256], memTb[:, c, mb * 128:(mb + 1) * 128], wmem[:, c, 256:512], start=(c == 0), stop=(c == 7))
        CP("act", mvt[:, mb, :], B1[:, 0:256])

    S.barrier()
    A.release(amark)
    wA = A.alloc("wA", [128, 8, 1792], BF16)
    xTa = [A.alloc(f"xTa{i}", [128, 8, 512], BF16) for i in range(2)]
    for c in range(8):
        DMA("pool", wA[:, c, :], w_a[c * 128:(c + 1) * 128, :])

    fm_banks = [B0, B1, B2, B3]
    tm_banks = [B4, B5, B6, B7]
    fmi = 0
    tmi = 0
    for tt in range(8):
        xb = xTa[tt % 2]
        DMA("pool", xb, V(xT.ap[:, tt * 512:(tt + 1) * 512].rearrange("(c p) t -> p c t", p=128), []))
        own = tt >= 4
        t0 = tt * 512
        groups = [("k", hh, 512 + hh * 128) for hh in range(4)]
        if own:
            groups += [("q", hh, hh * 128) for hh in range(4)] + [("m", cc, 1024 + cc * 128) for cc in range(2)]
        for kind, idx, col in groups:
            bk = fm_banks[fmi % 4]
            fmi += 1
            for c in range(8):
                MM(bk, wA[:, c, col:col + 128], xb[:, c, :], start=(c == 0), stop=(c == 7))
            if kind == "k":
                CP("act", KTc[:, idx, t0:t0 + 512].k(tt, idx), bk)
            elif kind == "q":
                qs = slice(t0 - 2048, t0 - 2048 + 512)
                CP("act", V(QTp[0].ap[0:64, idx, qs], [("QTp0", tt, idx)]), bk[0:64, :])
                CP("act", V(QTp[1].ap[64:128, idx, qs], [("QTp1", tt, idx)]), bk[64:128, :])
                MS("dve", V(QTp[0].ap[64:128, idx, qs], [("QTp0", tt, idx)]), 0.0)
                MS("dve", V(QTp[1].ap[0:64, idx, qs], [("QTp1", tt, idx)]), 0.0)
            else:
                CP("act", mqT[:, idx, t0 - 2048:t0 - 2048 + 512].k(tt, idx), bk)
        for j in range(4):
            bk = tm_banks[tmi % 4]
            tmi += 1
            for c in range(8):
                MM(bk, xb[:, c, j * 128:(j + 1) * 128], wA[:, c, 1280:1792], start=(c == 0), stop=(c == 7))
            CP("dve", Vt[:, tt * 4 + j, :].k(tt * 4 + j), bk)
    if H_LVL >= 70:
        S.barrier()
        A.release(amark)
        Dst = A.alloc("Dst", [128, 896], F32)
        Pt = [A.alloc(f"Pt{i}", [128, 512], BF16) for i in range(3)]
        sc = [A.alloc(f"sc{i}", [128, 512], F32) for i in range(2)]
        On = [A.alloc(f"On{i}", [128, 512], F32) for i in range(2)]
        Rz = A.alloc("Rz", [128, 512], F32)
        dsq = A.alloc("dsq", [128, 512], BF16)
        rstd = A.alloc("rstd", [128, 512], F32)
        DMA("sp", Dst, dstrip[:, 1536:2432])
        qaug = A.alloc("qaug", [128, 4, 512], BF16)
        sgn = A.alloc("sgn", [128, 256], BF16)
        btab = A.alloc("btab", [128, 4, 2, 32], F32)
        DMA("pool", qaug.re("p h i -> p (h i)"), qaug_d)
        DMA("pool", sgn, sgn_d)
        DMA("sp", btab.re("p h s d -> p (h s d)"), btab_d)
        Pt.append(A.alloc("Pt3", [128, 512], BF16))
        Pt.append(A.alloc("Pt4", [128, 512], BF16))
        Sbanks = [B0, B1, B7, B6]
        tiles = []
        oz = 0
        for h in range(4):
            for qt in range(4):
                q0 = 2048 + qt * 512
                for m in range(2):
                    Ob, Zb = (B2, B3) if oz % 2 == 0 else (B4, B5)
                    oz += 1
                    kbs = []
                    for kb in range(32):
                        k0 = kb * 128
                        if k0 + 127 < q0:
                            md = q0 - k0 - 127
                        elif k0 > q0 + 511:
                            md = k0 - q0 - 511
                        else:
                            md = 0
                        if SLOPES[h] * md <= ALIBI_THR:
                            kbs.append(kb)
                    for ii, kb in enumerate(kbs):
                        tiles.append(dict(h=h, qt=qt, m=m, kb=kb, first=(ii == 0), last=(ii == len(kbs) - 1),
                                          Ob=Ob, Zb=Zb))
        LOOK = 4

        def emit_front(i):
            t = tiles[i]
            h, qt, m, kb = t["h"], t["qt"], t["m"], t["kb"]
            k0 = kb * 128
            q0 = 2048 + qt * 512
            Sb_ = Sbanks[i % 4]
            lhs = V(KTc.ap[:, h, k0:k0 + 128], [("KTc", kb // 4, h)])
            rhs = V(QTp[m].ap[:, h, qt * 512:(qt + 1) * 512], [(f"QTp{m}", qt + 4, h)])
            if k0 + 128 <= q0 or k0 >= q0 + 512:
                left = k0 + 128 <= q0
                dist = (q0 - k0) // 128 if left else (k0 - q0) // 128
                MM(Sb_, lhs, rhs, start=True, stop=False)
                MM(Sb_, sgn[:, 0:128] if left else sgn[:, 128:256], qaug[:, h, :], start=False, stop=True)
                ACT(Pt[i % 5], Sb_, AF.Exp, bias=btab[:, h, 0 if left else 1, dist:dist + 1], scale=0.125)
            else:
                MM(Sb_, lhs, rhs)
                s0 = q0 - k0 + 1920 - 1536
                STT("dve", sc[i % 2], Dst[:, s0:s0 + 512], -8.0 * SLOPES[h], Sb_, ALU.mult, ALU.add)
                ACT(Pt[i % 5], sc[i % 2], AF.Exp, scale=0.125)

        def emit_back(i):
            t = tiles[i]
            h, qt, m, kb = t["h"], t["qt"], t["m"], t["kb"]
            Ob, Zb = t["Ob"], t["Zb"]
            pt = Pt[i % 5]
            MM(Ob, Vt[:, kb, h * 128:(h + 1) * 128].k(kb), pt, start=t["first"], stop=t["last"])
            MM(Zb, ones_b, pt, start=t["first"], stop=t["last"])
            if t["last"]:
                RECIP(Rz, Zb)
                TT("dve", On[m], Ob, Rz, ALU.mult)
                if m == 1:
                    STT("dve", On[0], On[1], lam_t[:, 0:1], On[0], ALU.mult, ALU.add)
                    TT("dve", dsq, On[0], On[0], ALU.mult)
                    deferred.append((i + 3, h, qt))

        deferred = []

        def run_deferred(i, force=False):
            while deferred and (force or deferred[0][0] <= i):
                _, h, qt = deferred.pop(0)
                MM(B6, ones_b, dsq)
                ACT(rstd, B6, AF.Ln, bias=eps_t, scale=1.0 / 128)
                ACT(rstd, rstd, AF.Exp, scale=-0.5)
                STT("dve", catD[:, h, qt * 512:(qt + 1) * 512].k(qt, h), On[0], dnw_t[:, 0:1], rstd,
                    ALU.mult, ALU.mult)

        for i in range(len(tiles) + LOOK):
            if i < len(tiles):
                emit_front(i)
            if i - LOOK >= 0:
                emit_back(i - LOOK)
                run_deferred(i - LOOK)
        run_deferred(0, force=True)
        si = 0
        pi = 0
        xg = 0
        for qt in range(4):
            for h in range(4):
                pr = (h % 2) * 64
                Ob, Zb = (B2, B3) if xg % 2 == 0 else (B4, B5)
                Rx = Rz if xg % 2 == 0 else rstd
                xg += 1
                for mb in range(2):
                    Sb_ = (B0, B1, B7, B6)[si % 4]
                    si += 1
                    pt = Pt[pi % 5]
                    pi += 1
                    MM(Sb_, mkT[pr:pr + 64, h // 2, mb * 128:(mb + 1) * 128],
                       V(mqT.ap[pr:pr + 64, h // 2, qt * 512:(qt + 1) * 512], [("mqT", qt + 4, h // 2)]))
                    ACT(pt, Sb_, AF.Exp, scale=0.125)
                    MM(Ob[0:64, :], mvt[:, mb, h * 64:(h + 1) * 64], pt, start=(mb == 0), stop=(mb == 1))
                    MM(Zb[0:64, :], ones_b[:, 0:64], pt, start=(mb == 0), stop=(mb == 1))
                RECIP(Rx[0:64, :], Zb[0:64, :])
                TT("dve", catM[0:64, h, qt * 512:(qt + 1) * 512].k(qt, h), Ob[0:64, :], Rx[0:64, :], ALU.mult)

    if dbg and stage == 2:
        S.barrier()
        DMA("pool", dbg_o[:, 0:8192], catD.re("p h t -> p (h t)"), final=True)
        DMA("pool", dbg_o[0:64, 8192:16384], catM[0:64].re("p h t -> p (h t)"), final=True)
        S.emit()
        return nc

    S.barrier()
    A.release(a4mark)
    woD = A.alloc("woD", [128, 4, 1024], BF16)
    woH = A.alloc("woH", [128, 8, 1024], BF16)
    lnt = A.alloc("lnt", [128, 4, 1024], F32)
    rw = A.alloc("rw", [128, 8, 32], F32)
    rb = A.alloc("rb", [128, 32], F32)
    carry = A.alloc("carry", [128, 32], F32)
    xt = [A.alloc(f"xt{i}", [128, 1024], F32) for i in range(2)]
    zt = [A.alloc(f"zt{i}", [128, 1024], F32) for i in range(2)]
    hb = [A.alloc(f"hb{i}", [128, 1024], BF16) for i in range(2)]
    def a4set(i):
        d = {}
        d["hT"] = A.alloc(f"hT{i}", [128, 8, 128], F32)
        d["bst"] = A.alloc(f"bst{i}", [128, 2, 6], F32)
        d["mv_"] = A.alloc(f"mv_{i}", [128, 4], F32)
        for nm, w in (("lg", 32), ("v8", 8), ("negv0", 1), ("e4", 4), ("s4", 2), ("msk", 32), ("eg", 32),
                      ("rnk", 32), ("slm", 32), ("ovf", 32), ("oh", 32), ("slf", 4)):
            d[nm] = A.alloc(f"{nm}{i}", [128, w], F32)
        d["mskb"] = A.alloc(f"mskb{i}", [128, 32], BF16)
        return d
    a4 = [a4set(0), a4set(1)]
    bst = a4[0]["bst"]
    mv_ = a4[0]["mv_"]
    ecap = cst_f[:, 904:936]
    tsf = A.alloc("tsf", [128, 16, 4], F32) if dbg else None

    for hh in range(4):
        DMA("pool", woD[:, hh, :], w_o[hh * 128:(hh + 1) * 128, :])
    for i in range(8):
        DMA("pool", woH[0:64, i, :].k(i), w_o[512 + i * 64:512 + (i + 1) * 64, :])
        MS("dve", woH[64:128, i, :].k(i), 0.0)
    DMA("sp", lnt, V(lnp.ap.unsqueeze(0).broadcast_to([128, 4, 1024]), []))
    DMA("sp", rw, V(router_w.ap.rearrange("(c p) e -> p c e", p=128), []))
    DMA("sp", rb, V(router_b.ap.broadcast_to([128, 32]), []))
    MS("dve", carry, 0.0)
    zpad = A.alloc("zpad", [128, 3, 1024], BF16)
    MS("pool", zpad, 0.0)
    for e_ in range(NEXP):
        DMA("sp" if e_ % 2 == 0 else "act",
            V(Xs.ap[e_ * CAP:(e_ + 1) * CAP, :].rearrange("(r p) d -> p r d", p=128), [("Xs", "z", e_)]), zpad)
    xs_ready = A.alloc("xs_ready", [128, 8], F32)
    S.op("dve", lambda e: e.memset(xs_ready.ap, 0.0), [("Xs", "z", e_) for e_ in range(NEXP)], ["Xs_ready"])

    def layer_norm(dst, src, gi, bst=None, mv_=None):
        bst = bst if bst is not None else ln_scratch[0]
        mv_ = mv_ if mv_ is not None else ln_scratch[1]
        for c2 in range(2):
            S.op("dve", lambda e, c2=c2, bst=bst, src=src: e.bn_stats(out=bst.ap[:, c2, :],
                                                                      in_=src.ap[:, c2 * 512:(c2 + 1) * 512]),
                 src.keys, bst.keys)
        S.op("dve", lambda e, bst=bst, mv_=mv_: e.bn_aggr(out=mv_.ap[:, 0:2], in_=bst.ap), bst.keys, mv_.keys)
        ACT(mv_[:, 2:3], mv_[:, 1:2], AF.Ln, bias=eps_t, scale=1.0)
        ACT(mv_[:, 2:3], mv_[:, 2:3], AF.Exp, scale=-0.5)
        TS("dve", dst, src, mv_[:, 0:1], ALU.subtract, mv_[:, 2:3], ALU.mult)
        TT("dve", dst, dst, lnt[:, gi, :], ALU.mult)
        TT("dve", dst, dst, lnt[:, gi + 1, :], ALU.add)

    ln_scratch = [bst, mv_]
    NT = int(os.environ.get("A4_NT", "16"))
    for ti in range(NT):
        t0 = ti * 128
        P_ = a4[ti % 2]
        hT, lg, v8, negv0, e4, s4, msk, mskb, eg, rnk, slm, ovf, oh, slf = (
            P_["hT"], P_["lg"], P_["v8"], P_["negv0"], P_["e4"], P_["s4"], P_["msk"], P_["mskb"], P_["eg"],
            P_["rnk"], P_["slm"], P_["ovf"], P_["oh"], P_["slf"])
        x_t = xt[ti % 2]
        z_t = zt[ti % 2]
        h_b = hb[ti % 2]
        DMA("sp", x_t, x_own[t0:t0 + 128, :])
        for half in range(2):
            bk = (B0, B1)[half] if ti % 2 == 0 else (B2, B3)[half]
            cs = slice(half * 512, (half + 1) * 512)
            n = 0
            for hh in range(4):
                MM(bk, V(catD.ap[:, hh, t0:t0 + 128], []), woD[:, hh, cs], start=(n == 0), stop=False)
                n += 1
            for hh in range(4):
                MM(bk, V(ohg.ap[:, hh, t0:t0 + 128], []), woH[:, hh, cs].k(hh), start=False, stop=False)
            for hh in range(4):
                MM(bk, V(catM.ap[:, hh, t0:t0 + 128], []), woH[:, 4 + hh, cs].k(4 + hh), start=False, stop=(hh == 3))
            STT("dve", z_t[:, cs], x_t[:, cs], ALPHA, bk, ALU.mult, ALU.add)
        layer_norm(z_t, z_t, 0, P_["bst"], P_["mv_"])
        DMA("sp", V(Hf.ap[t0:t0 + 128, :], [("Hf", ti)]), z_t)
        CP("act", h_b, z_t)
        for c in range(8):
            TR((B4 if c < 4 else B5)[:, (c % 4) * 128:(c % 4 + 1) * 128], z_t[:, c * 128:(c + 1) * 128], ident_f)
        CP("act", hT[:, 0:4, :].re("p c t -> p (c t)"), B4)
        CP("act", hT[:, 4:8, :].re("p c t -> p (c t)"), B5)
        for c in range(8):
            MM(B6[:, 0:32], hT[:, c, :], rw[:, c, :], start=(c == 0), stop=(c == 7))
        TT("dve", lg, B6[:, 0:32], rb, ALU.add)
        S.op("dve", lambda e, v8=v8, lg=lg: e.max(out=v8.ap, in_=lg.ap), lg.keys, v8.keys)
        TS("dve", negv0, v8[:, 0:1], -1.0, ALU.mult)
        ACT(e4, v8[:, 0:4], AF.Exp, bias=negv0)
        S.op("dve", lambda e, s4=s4, e4=e4: e.reduce_sum(out=s4.ap[:, 0:1], in_=e4.ap, axis=AX.X), e4.keys, s4.keys)
        RECIP(s4[:, 1:2], s4[:, 0:1])
        TS("dve", tokgate[:, ti, :].k(ti), e4, s4[:, 1:2], ALU.mult)
        TS("dve", msk, lg, v8[:, 3:4], ALU.is_ge)
        ACT(eg, lg, AF.Exp, bias=negv0)
        STT("dve", tokG[:, ti, :].k(ti), eg, s4[:, 1:2], msk, ALU.mult, ALU.mult)
        CP("dve", mskb, msk)
        MM(B7[:, 0:32], tri_b, mskb)
        MM(B7[:, 32:64], ones_b, mskb)
        TT("dve", rnk, B7[:, 0:32], carry, ALU.add)
        TT("dve", carry, B7[:, 32:64], carry, ALU.add)
        TT("dve", slm, rnk, ecap, ALU.add)
        TS("dve", ovf, rnk, float(CAP), ALU.is_gt, 1.0e6, ALU.mult)
        TT("dve", slm, slm, ovf, ALU.add)
        for k in range(4):
            TS("dve", oh, lg, v8[:, k:k + 1], ALU.is_equal)
            TT("dve", oh, oh, slm, ALU.mult)
            S.op("dve", lambda e, k=k, slf=slf, oh=oh: e.reduce_sum(out=slf.ap[:, k:k + 1], in_=oh.ap, axis=AX.X),
                 oh.keys, slf.keys)
        for k in range(4):
            CP("dve", tokslot[ti][k], slf[:, k:k + 1])
        if dbg:
            CP("dve", tsf[:, ti, :].k(ti), slf)
        for k in range(4):
            S.dma("pool", lambda e, ti=ti, k=k, h_b=h_b: e.indirect_dma_start(
                out=Xs.ap, out_offset=bass.IndirectOffsetOnAxis(ap=tokslot[ti][k].ap, axis=0),
                in_=h_b.ap, in_offset=None, bounds_check=bcheck(e), oob_is_err=False),
                list(h_b.keys) + list(tokslot[ti][k].keys) + ["Xs_ready"], [("Xs", "s", ti, k)])

    if dbg and stage == 3:
        S.barrier()
        for ti in range(NT):
            DMA("sp", dbg_o[:, ti * 64:ti * 64 + 32], tokG[:, ti, :], final=True)
            DMA("sp", dbg_o[:, ti * 64 + 32:ti * 64 + 36], tokgate[:, ti, :], final=True)
            DMA("sp", dbg_o[:, ti * 64 + 36:ti * 64 + 40], tsf[:, ti, :], final=True)
        for ti in range(16):
            DMA("sp", out[ti * 128:(ti + 1) * 128, :], Hf[ti * 128:(ti + 1) * 128, :], final=True)
        S.emit()
        return nc

    S.barrier()
    A.release(pM)
    lnt = A.alloc("lnt2", [128, 4, 1024], F32)
    bst = A.alloc("bst2", [128, 2, 6], F32)
    mv_ = A.alloc("mv2_", [128, 4], F32)
    ln_scratch = [bst, mv_]
    DMA("sp", lnt, V(lnp.ap.unsqueeze(0).broadcast_to([128, 4, 1024]), []))
    wgu = [A.alloc(f"wgu{i}", [128, 8, 2048], BF16) for i in range(2)]
    wdn = [A.alloc(f"wdn{i}", [128, 8, 1024], BF16) for i in range(2)]
    bgu = A.alloc("bgu", [128, NEXP, 2, 8], F32)
    xs = A.alloc("xs", [128, 3, 1024], BF16)
    xsT = A.alloc("xsT", [128, 8, CAP], BF16)
    actT = A.alloc("actT", [128, 8, CAP], BF16)
    gt = [A.alloc(f"gt{i}", [128, CAP], F32) for i in range(2)]
    lt = [A.alloc(f"lt{i}", [128, CAP], F32) for i in range(2)]
    sg = [A.alloc(f"sg{i}", [128, CAP], F32) for i in range(2)]
    yb = [A.alloc(f"yb{i}", [128, 1024], BF16) for i in range(2)]
    DMA("sp", bgu.re("p e j c -> p (e j c)"), b_gu)
    NE = int(os.environ.get("M_NE", str(NEXP)))
    B3b_ = B3.bitcast(BF16)
    B7b_ = B7.bitcast(BF16)
    yi = 0
    def load_expert(ee):
        for c in range(8):
            DMA("pool", wgu[ee % 2][:, c, :].k(c), w_gu[ee, c * 128:(c + 1) * 128, :])
        for c in range(8):
            DMA("pool", wdn[ee % 2][:, c, :].k(c), w_dn[ee, c * 128:(c + 1) * 128, :])

    xs2 = [xs, A.alloc("xs_b", [128, 3, 1024], BF16)]
    xsT2 = [xsT, A.alloc("xsT_b", [128, 8, CAP], BF16)]

    def load_xs(ee):
        DMA("sp", xs2[ee % 2], V(Xs.ap[ee * CAP:(ee + 1) * CAP, :].rearrange("(r p) d -> p r d", p=128), Xs.keys))

    def transpose_xs(ee):
        for r in range(3):
            tb = B3b_ if r % 2 == 0 else B7b_
            for c in range(8):
                TR(tb[:, c * 128:(c + 1) * 128], xs2[ee % 2][:, r, c * 128:(c + 1) * 128], ident_b)
            CP("act" if r % 2 == 0 else "dve", xsT2[ee % 2][:, :, r * 128:(r + 1) * 128],
               tb.re("p (c t) -> p c t", c=8))

    load_expert(0)
    load_xs(0)
    transpose_xs(0)
    for e_ in range(NE):
        wg = wgu[e_ % 2]
        wd = wdn[e_ % 2]
        xsT = xsT2[e_ % 2]
        if e_ + 1 < NE:
            load_expert(e_ + 1)
            load_xs(e_ + 1)
        for fc in range(8):
            Bg = B0 if fc % 2 == 0 else B2
            Bl = B1 if fc % 2 == 0 else B4
            for c in range(8):
                MM(Bg[:, 0:CAP], wg[:, c, fc * 128:(fc + 1) * 128].k(c), xsT[:, c, :], start=(c == 0), stop=(c == 7))
            for c in range(8):
                MM(Bl[:, 0:CAP], wg[:, c, 1024 + fc * 128:1024 + (fc + 1) * 128].k(c), xsT[:, c, :],
                   start=(c == 0), stop=(c == 7))
            g_, l_, s_ = gt[fc % 2], lt[fc % 2], sg[fc % 2]
            TS("dve", g_, Bg[:, 0:CAP], bgu[:, e_, 0, fc:fc + 1], ALU.add, 7.0, ALU.min)
            TS("dve", l_, Bl[:, 0:CAP], bgu[:, e_, 1, fc:fc + 1], ALU.add, 7.0, ALU.min)
            ACT(s_, g_, AF.Sigmoid, scale=1.702)
            TS("dve", l_, l_, -7.0, ALU.max, 1.0, ALU.add)
            TT("dve", l_, l_, g_, ALU.mult)
            TT("dve", actT[:, fc, :], l_, s_, ALU.mult)
        if e_ + 1 < NE:
            transpose_xs(e_ + 1)
        for r in range(3):
            y_ = yb[yi % 2]
            yi += 1
            for half in range(2):
                By = B5 if half == 0 else B6
                for fc in range(8):
                    MM(By, actT[:, fc, r * 128:(r + 1) * 128], wd[:, fc, half * 512:(half + 1) * 512].k(fc),
                       start=(fc == 0), stop=(fc == 7))
                CP("act", y_[:, half * 512:(half + 1) * 512], By)
            DMA("sp", V(Ys.ap[e_ * CAP + r * 128:e_ * CAP + (r + 1) * 128, :], [("Ys", e_, r)]), y_)

    S.barrier()
    cmark = A.mark()
    yk = [A.alloc(f"yk{i}", [128, 4, 1024], BF16) for i in range(2)]
    hf = [A.alloc(f"hf{i}", [128, 1024], F32) for i in range(2)]
    bdn_t = A.alloc("bdn_t", [32, 1024], F32)
    GTs = [A.alloc(f"GT{i}", [32, 128], F32) for i in range(2)]
    DMA("sp", bdn_t, b_dn)

    def comb_a(ti):
        t0 = ti * 128
        yk_ = yk[ti % 2]
        h_ = hf[ti % 2]
        GT = GTs[ti % 2]
        S.op("act", lambda e, yk_=yk_: e.memzero(yk_.ap), (), [(yk_.keys[0], k) for k in range(4)])
        for k in range(4):
            S.dma("pool", lambda e, ti=ti, k=k, yk_=yk_: e.indirect_dma_start(
                out=yk_.ap[:, k, :], out_offset=None,
                in_=Ys.ap, in_offset=bass.IndirectOffsetOnAxis(ap=tokslot[ti][k].ap, axis=0),
                bounds_check=bcheck(e), oob_is_err=False),
                list(tokslot[ti][k].keys), [(yk_.keys[0], k)])
        DMA("sp", h_, V(Hf.ap[t0:t0 + 128, :], [("Hf", ti)]))
        TR(B4[0:32, 0:128], tokG[:, ti, :].k(ti), ident_f)
        CP("act", GT, B4[0:32, 0:128])
        for half in range(2):
            bk = (B0, B1)[half] if ti % 2 == 0 else (B2, B3)[half]
            cs = slice(half * 512, (half + 1) * 512)
            MM(bk, GT, bdn_t[:, cs])

    def comb_b(ti):
        t0 = ti * 128
        yk_ = yk[ti % 2]
        h_ = hf[ti % 2]
        for half in range(2):
            bk = (B0, B1)[half] if ti % 2 == 0 else (B2, B3)[half]
            cs = slice(half * 512, (half + 1) * 512)
            STT("dve", h_[:, cs], h_[:, cs], ALPHA, bk, ALU.mult, ALU.add)
        for k in range(4):
            STT("dve", h_, yk_[:, k, :].k(k), tokgate[:, ti, k:k + 1].k(ti), h_, ALU.mult, ALU.add)
        layer_norm(h_, h_, 2)
        DMA("sp", V(out.ap[t0:t0 + 128, :], [("out", ti)]), h_, final=True)

    comb_a(0)
    for ti in range(16):
        if ti + 1 < 16:
            comb_a(ti + 1)
        comb_b(ti)

    S.emit()
    return nc


def make_consts():
    c = np.zeros((128, 1024), np.float32)
    r = np.arange(128)[:, None]
    cc = np.arange(128)[None, :]
    same = (r // 16) == (cc // 16)
    c[:, 0:128] = (same & (r <= cc))
    c[:, 128:256] = (same & (r >= cc))
    c[:, 256:384] = (same & (r < cc))
    c[:, 384:512] = (same & (r > cc))
    c[:, 512:640] = np.eye(128)
    c[:, 640:768] = (r <= cc)
    c[:, 768:776] = (r // 16) == np.arange(8)[None, :]
    c[:, 776:904] = 1.0
    c[:, 904:936] = (np.arange(NEXP) * CAP - 1)[None, :]
    return c


def make_alibi_tabs():
    slopes = [2.0 ** (-8.0 * (h + 1) / 4) for h in range(4)]
    i = np.arange(512)
    qaug = np.zeros((2, 4, 512), np.float32)
    for h, m in enumerate(slopes):
        qaug[0, h] = -8.0 * m * 16.0 * (i // 16)
        qaug[1, h] = -8.0 * m * (i % 16)
    j = np.arange(128)[:, None]
    dist = np.arange(32)[None, :]
    btab = np.zeros((128, 4, 2, 32), np.float32)
    for h, m in enumerate(slopes):
        btab[:, h, 0, :] = m * j - m * 128.0 * dist
        btab[:, h, 1, :] = -m * j - m * 128.0 * dist
    sgn = np.zeros((128, 256), np.float32)
    sgn[0:2, 0:128] = 1.0
    sgn[0:2, 128:256] = -1.0
    qa = np.zeros((128, 2048), np.float32)
    qa[0:2] = qaug.reshape(2, 2048)
    return qa, btab.reshape(128, 256), sgn


def make_dstrip():
    j = np.arange(128)[:, None]
    x = np.arange(6016)[None, :]
    return np.abs(x - j - 1920).astype(np.float32)


def prep_inputs(inp):
    x = np.asarray(inp["x"], np.float32)
    mem = np.asarray(inp["mem"], np.float32)
    w_in = np.asarray(inp["w_in"], np.float32)[0]
    dq, dk, dv = w_in[:, 0:512], w_in[:, 512:1024], w_in[:, 1024:1536]
    hq, hff, hfb, hi, hg, mq = (w_in[:, 1536 + 256 * i:1536 + 256 * (i + 1)] for i in range(6))
    w_a = np.ascontiguousarray(np.concatenate([dq, dk, mq, dv], axis=1))
    lbf = np.asarray(inp["hgrn_lb_fwd"], np.float32)
    lbb = np.asarray(inp["hgrn_lb_bwd"], np.float32)
    w_gu_full = np.asarray(inp["w_gate_up"], np.float32)[0]
    w_gu = np.ascontiguousarray(np.concatenate([w_gu_full[:, :, 0::2], w_gu_full[:, :, 1::2]], axis=2))
    bgu = np.asarray(inp["b_gate_up"], np.float32)[0]
    bt = bgu.reshape(NEXP, 8, 128, 2).transpose(2, 0, 3, 1)
    b_gu = np.ascontiguousarray(bt.reshape(128, NEXP * 16))
    shared = {
        "lamv": np.stack([inp["lam_q1"][0], inp["lam_k1"][0], inp["lam_q2"][0], inp["lam_k2"][0]]).astype(np.float32),
        "hnw": np.ascontiguousarray(np.asarray(inp["hgrn_norm_w"], np.float32)[0].reshape(4, 64).T),
        "dnw": np.asarray(inp["diff_norm_w"], np.float32)[0].reshape(128, 1).copy(),
        "w_mem": np.asarray(inp["w_mem_kv"], np.float32)[0],
        "w_o": np.asarray(inp["w_o"], np.float32)[0],
        "lnp": np.stack([inp["ln1_g"][0], inp["ln1_b"][0], inp["ln2_g"][0], inp["ln2_b"][0]]).astype(np.float32),
        "router_w": np.asarray(inp["router_w"], np.float32)[0],
        "router_b": np.asarray(inp["router_b"], np.float32)[0].reshape(1, 32).copy(),
        "w_gu": w_gu, "b_gu": b_gu,
        "w_dn": np.asarray(inp["w_down"], np.float32)[0],
        "b_dn": np.asarray(inp["b_down"], np.float32)[0],
        "w_a": w_a,
        "cst": make_consts(),
        "dstrip": make_dstrip(),
        "qaug": make_alibi_tabs()[0], "btab": make_alibi_tabs()[1], "sgn": make_alibi_tabs()[2],
    }
    in_maps = []
    for c in range(NCORES):
        b, hf = c // 2, c % 2
        seq = x[b] if hf == 1 else x[b, ::-1]
        if hf == 1:
            hA, hB, lA, lB = hff, hfb, lbf, lbb
        else:
            hA, hB, lA, lB = hfb, hff, lbb, lbf
        m = dict(shared)
        m["xT"] = np.ascontiguousarray(seq.T)
        m["x_own"] = np.ascontiguousarray(seq[2048:])
        m["memT"] = np.ascontiguousarray(mem[b].T)
        m["w_h"] = np.ascontiguousarray(np.concatenate([hA, hi, hq, hB, hg], axis=1))
        m["lbp"] = np.ascontiguousarray(np.stack([lA[0], lA[1], lB[0], lB[1]]))
        in_maps.append(m)
    return in_maps


_NC_CACHE = {}


def kernel(**inputs):
    in_maps = prep_inputs(inputs)
    if "nc" not in _NC_CACHE:
        _NC_CACHE["nc"] = build_program()
    nc = _NC_CACHE["nc"]
    res = run_bass_kernel_spmd(nc, in_maps, core_ids=list(range(NCORES)))
    out = np.zeros((4, 4096, 1024), np.float32)
    for c in range(NCORES):
        b, hf = c // 2, c % 2
        o = np.asarray(res.results[c]["out"], np.float32)
        if hf == 1:
            out[b, 2048:] = o
        else:
            out[b, 0:2048] = o[::-1]
    return out
```
